# Optimizing a Trainium2 kernel written in Bass

```python
import math
import jax, jax.numpy as jnp
from jax import lax
import numpy as np

D_MODEL = 1024
BATCH = 16
SEQ = 2048
DEPTH = 2

HEAD_DIM = 64
CONV_CH = 256
CONV_WIDTH = 31
DSA_HEADS = 6
MOBA_HEADS = 6
DSA_W = DSA_HEADS * HEAD_DIM
MOBA_W = MOBA_HEADS * HEAD_DIM
MIX_W = CONV_CH + DSA_W + MOBA_W
IDX_HEADS = 4
IDX_DIM = 32
DSA_TOPK = 256
DSA_Q_BLOCK = 128
MOBA_BLOCK = 256
MOBA_TOPK = 3
MOBA_Q_BLOCK = 16
ROPE_THETA = 500000.0
ROPE_FRACTION = 4
N_EXPERTS = 16
N_EXPERT_GROUPS = 4
EXPERTS_PER_GROUP = N_EXPERTS // N_EXPERT_GROUPS
TOP_K = 2
D_EXPERT = 256
EPS = 1e-6
IN_SPLITS = (CONV_CH, CONV_CH, DSA_W, HEAD_DIM, HEAD_DIM, IDX_HEADS * IDX_DIM, IDX_DIM, IDX_HEADS, MOBA_W, MOBA_W, MOBA_W)
IN_W = 2 * CONV_CH + DSA_W + 2 * HEAD_DIM + IDX_HEADS * IDX_DIM + IDX_DIM + IDX_HEADS + 3 * MOBA_W

kernel_name = 'hybrid_conv_dsa_moba_moe_block'

F32 = jnp.float32


def rms_norm(x, g):
    xf = x.astype(F32)
    y = xf * lax.rsqrt(jnp.mean(xf * xf, axis=-1, keepdims=True) + EPS)
    return (y * g.astype(F32)).astype(x.dtype)


def modulate(h, shift, scale):
    return h * (1 + scale[:, None, :]) + shift[:, None, :]


def rope_partial(x, pos):
    d = x.shape[-1]
    rot = d // ROPE_FRACTION
    half = rot // 2
    inv = jnp.exp(-math.log(ROPE_THETA) * (jnp.arange(half, dtype=F32) * 2.0 / rot))
    ang = pos.astype(F32)[:, None] * inv[None, :]
    cos = jnp.cos(ang)[:, None, :]
    sin = jnp.sin(ang)[:, None, :]
    xf = x.astype(F32)
    x1 = xf[..., :half]
    x2 = xf[..., half:rot]
    out = jnp.concatenate([x1 * cos - x2 * sin, x2 * cos + x1 * sin, xf[..., rot:]], axis=-1)
    return out.astype(x.dtype)


def conformer_conv(u, g, w_dw, b_dw, ln_g, ln_b):
    a = u * jax.nn.sigmoid(g)
    y = lax.conv_general_dilated(a, w_dw[:, None, :].astype(a.dtype), window_strides=(1,),
                                 padding=((CONV_WIDTH - 1, 0),),
                                 dimension_numbers=('NWC', 'WIO', 'NWC'),
                                 feature_group_count=CONV_CH)
    y = (y + b_dw).astype(F32)
    mu = jnp.mean(y, axis=-1, keepdims=True)
    var = jnp.mean(jnp.square(y - mu), axis=-1, keepdims=True)
    y = (y - mu) * lax.rsqrt(var + EPS) * ln_g.astype(F32) + ln_b.astype(F32)
    return jax.nn.silu(y).astype(u.dtype)


def dsa_attention(q, k, v, qi, ki, wi):
    B, L, H, dh = q.shape
    topk = min(DSA_TOPK, L // 4)
    nq = L // DSA_Q_BLOCK
    key_pos = jnp.arange(L)
    ki32 = ki.astype(F32)
    scale = dh ** -0.5

    def blocks(a):
        return jnp.moveaxis(a.reshape((B, nq, DSA_Q_BLOCK) + a.shape[2:]), 1, 0)

    def one_block(args):
        qb, qib, wib, t0 = args
        tq = t0 + jnp.arange(DSA_Q_BLOCK)
        logits = jnp.einsum('bqhd,bsd->bqhs', qib.astype(F32), ki32) * (IDX_DIM ** -0.5)
        score = jnp.einsum('bqh,bqhs->bqs', wib.astype(F32) * (IDX_HEADS ** -0.5), jax.nn.relu(logits))
        causal = key_pos[None, :] <= tq[:, None]
        score = jnp.where(causal[None], score, -jnp.inf)
        _, idx = lax.top_k(score, topk)
        valid = idx <= tq[None, :, None]
        k_sel = jax.vmap(lambda kk, ii: kk[ii])(k, idx)
        v_sel = jax.vmap(lambda vv, ii: vv[ii])(v, idx)
        s = jnp.einsum('bqhd,bqkd->bqhk', qb, k_sel).astype(F32) * scale
        s = jnp.where(valid[:, :, None, :], s, -jnp.inf)
        p = jax.nn.softmax(s, axis=-1).astype(v.dtype)
        return jnp.einsum('bqhk,bqkd->bqhd', p, v_sel)

    t0s = jnp.arange(nq, dtype=jnp.int32) * DSA_Q_BLOCK
    out = lax.map(one_block, (blocks(q), blocks(qi), blocks(wi), t0s))
    return jnp.moveaxis(out, 0, 1).reshape(B, L, H * dh)


def moba_attention(q, k, v):
    B, L, H, dh = q.shape
    nb = -(-L // MOBA_BLOCK)
    n_sel = min(MOBA_TOPK, nb - 1)
    pad = nb * MOBA_BLOCK - L
    scale = dh ** -0.5

    def to_blocks(a):
        a = jnp.pad(a, ((0, 0), (0, pad), (0, 0), (0, 0)))
        return a.reshape(B, nb, MOBA_BLOCK, H, dh).transpose(0, 3, 1, 2, 4)

    kb = to_blocks(k)
    vb = to_blocks(v)
    k_mean = jnp.mean(kb.astype(F32), axis=3)
    nq = L // MOBA_Q_BLOCK
    b_i = jnp.arange(B)[:, None, None, None]
    h_i = jnp.arange(H)[None, None, :, None]
    blk_ids = jnp.arange(nb)
    in_blk = jnp.arange(MOBA_BLOCK)

    def one_block(args):
        qb, t0 = args
        tq = t0 + jnp.arange(MOBA_Q_BLOCK)
        own = t0 // MOBA_BLOCK
        k_own = lax.dynamic_index_in_dim(kb, own, axis=2, keepdims=False)
        v_own = lax.dynamic_index_in_dim(vb, own, axis=2, keepdims=False)
        own_mask = (own * MOBA_BLOCK + in_blk)[None, :] <= tq[:, None]
        s_own = jnp.einsum('bqhd,bhkd->bqhk', qb, k_own).astype(F32) * scale
        s_own = jnp.where(own_mask[None, :, None, :], s_own, -jnp.inf)
        if n_sel == 0:
            p_own = jax.nn.softmax(s_own, axis=-1).astype(v.dtype)
            return jnp.einsum('bqhk,bhkd->bqhd', p_own, v_own)
        gate = jnp.einsum('bqhd,bhnd->bqhn', qb.astype(F32), k_mean)
        gate = jnp.where(blk_ids < own, gate, -jnp.inf)
        _, sel = lax.top_k(gate, n_sel)
        valid = sel < own
        k_sel = kb[b_i, h_i, sel]
        v_sel = vb[b_i, h_i, sel]
        s_past = jnp.einsum('bqhd,bqhnkd->bqhnk', qb, k_sel).astype(F32) * scale
        s_past = jnp.where(valid[..., None], s_past, -jnp.inf)
        s = jnp.concatenate([s_past.reshape(B, MOBA_Q_BLOCK, H, n_sel * MOBA_BLOCK), s_own], axis=-1)
        p = jax.nn.softmax(s, axis=-1).astype(v.dtype)
        p_past = p[..., :n_sel * MOBA_BLOCK].reshape(B, MOBA_Q_BLOCK, H, n_sel, MOBA_BLOCK)
        p_own = p[..., n_sel * MOBA_BLOCK:]
        return (jnp.einsum('bqhnk,bqhnkd->bqhd', p_past, v_sel)
                + jnp.einsum('bqhk,bhkd->bqhd', p_own, v_own))

    q_blocks = jnp.moveaxis(q.reshape(B, nq, MOBA_Q_BLOCK, H, dh), 1, 0)
    t0s = jnp.arange(nq, dtype=jnp.int32) * MOBA_Q_BLOCK
    out = lax.map(one_block, (q_blocks, t0s))
    return jnp.moveaxis(out, 0, 1).reshape(B, L, H * dh)


def token_mixers(h, pos, w_in, w_dw, b_dw, ln_g, ln_b, w_o):
    B, L, _ = h.shape
    proj = h @ w_in
    offs = np.cumsum(IN_SPLITS)[:-1].tolist()
    cu, cg, dq, dk, dv, iq, ik, iw, mq, mk, mv = jnp.split(proj, offs, axis=-1)
    y_conv = conformer_conv(cu, cg, w_dw, b_dw, ln_g, ln_b)
    dq = rope_partial(dq.reshape(B, L, DSA_HEADS, HEAD_DIM), pos)
    dk = rope_partial(dk[:, :, None, :], pos)[:, :, 0, :]
    iq = rope_partial(iq.reshape(B, L, IDX_HEADS, IDX_DIM), pos)
    ik = rope_partial(ik[:, :, None, :], pos)[:, :, 0, :]
    y_dsa = dsa_attention(dq, dk, dv, iq, ik, iw)
    mq = rope_partial(mq.reshape(B, L, MOBA_HEADS, HEAD_DIM), pos)
    mk = rope_partial(mk.reshape(B, L, MOBA_HEADS, HEAD_DIM), pos)
    mv = mv.reshape(B, L, MOBA_HEADS, HEAD_DIM)
    y_moba = moba_attention(mq, mk, mv)
    return jnp.concatenate([y_conv, y_dsa, y_moba], axis=-1) @ w_o


def grouped_moe(h, w_router, b_router, w_gate, w_up, w_down):
    B, L, D = h.shape
    aff = jax.nn.sigmoid(jnp.einsum('bld,de->ble', h.astype(F32), w_router.astype(F32)))
    biased = aff + b_router.astype(F32)
    grp = biased.reshape(B, L, N_EXPERT_GROUPS, EXPERTS_PER_GROUP)
    grp_score = jnp.sum(lax.top_k(grp, TOP_K)[0], axis=-1)
    best = jnp.argmax(grp_score, axis=-1)
    in_grp = (jnp.arange(N_EXPERTS) // EXPERTS_PER_GROUP) == best[..., None]
    _, top_idx = lax.top_k(jnp.where(in_grp, biased, -jnp.inf), TOP_K)
    w = jnp.take_along_axis(aff, top_idx, axis=-1)
    w = w / jnp.sum(w, axis=-1, keepdims=True)
    gates = jnp.sum(jax.nn.one_hot(top_idx, N_EXPERTS, dtype=F32) * w[..., None], axis=-2).astype(h.dtype)
    out = jnp.zeros_like(h)
    for e in range(N_EXPERTS):
        a = jax.nn.silu(h @ w_gate[e]) * (h @ w_up[e])
        out = out + gates[..., e:e + 1] * (a @ w_down[e])
    return out


def setup_inputs(seed: int = 0) -> dict:
    key = jax.random.key(seed)
    ks = jax.random.split(key, 18)

    def nrm(k, shape, s):
        return jax.random.normal(k, shape, F32) * s

    return {
        'x': nrm(ks[0], (BATCH, SEQ, D_MODEL), 1.0),
        'c': nrm(ks[1], (BATCH, D_MODEL), 1.0),
        'w_ada': nrm(ks[2], (DEPTH, D_MODEL, 6 * D_MODEL), 0.5 * D_MODEL ** -0.5),
        'b_ada': nrm(ks[3], (DEPTH, 6 * D_MODEL), 0.02),
        'g_mix': 1.0 + nrm(ks[4], (DEPTH, D_MODEL), 0.02),
        'w_in': nrm(ks[5], (DEPTH, D_MODEL, IN_W), D_MODEL ** -0.5),
        'w_dw': nrm(ks[6], (DEPTH, CONV_WIDTH, CONV_CH), CONV_WIDTH ** -0.5),
        'b_dw': nrm(ks[7], (DEPTH, CONV_CH), 0.02),
        'ln_conv_g': 1.0 + nrm(ks[8], (DEPTH, CONV_CH), 0.02),
        'ln_conv_b': nrm(ks[9], (DEPTH, CONV_CH), 0.02),
        'w_o': nrm(ks[10], (DEPTH, MIX_W, D_MODEL), MIX_W ** -0.5),
        'g_ffn': 1.0 + nrm(ks[11], (DEPTH, D_MODEL), 0.02),
        'w_router': nrm(ks[12], (D_MODEL, N_EXPERTS), D_MODEL ** -0.5),
        'b_router': nrm(ks[13], (N_EXPERTS,), 0.01),
        'w_gate': nrm(ks[14], (DEPTH, N_EXPERTS, D_MODEL, D_EXPERT), D_MODEL ** -0.5),
        'w_up': nrm(ks[15], (DEPTH, N_EXPERTS, D_MODEL, D_EXPERT), D_MODEL ** -0.5),
        'w_down': nrm(ks[16], (DEPTH, N_EXPERTS, D_EXPERT, D_MODEL), D_EXPERT ** -0.5),
        'g_final': 1.0 + nrm(ks[17], (D_MODEL,), 0.02),
    }


def reference(x, c, w_ada, b_ada, g_mix, w_in, w_dw, b_dw, ln_conv_g, ln_conv_b, w_o, g_ffn,
              w_router, b_router, w_gate, w_up, w_down, g_final):
    L = x.shape[1]
    pos = jnp.arange(L)
    c_act = jax.nn.silu(c)
    for l in range(DEPTH):
        mod = c_act @ w_ada[l] + b_ada[l]
        sh1, sc1, g1, sh2, sc2, g2 = jnp.split(mod, 6, axis=-1)
        h = modulate(rms_norm(x, g_mix[l]), sh1, sc1)
        x = x + g1[:, None, :] * token_mixers(h, pos, w_in[l], w_dw[l], b_dw[l], ln_conv_g[l], ln_conv_b[l], w_o[l])
        h = modulate(rms_norm(x, g_ffn[l]), sh2, sc2)
        x = x + g2[:, None, :] * grouped_moe(h, w_router, b_router, w_gate[l], w_up[l], w_down[l])
    return rms_norm(x, g_final)
```

```python
import contextlib
import math
import numpy as np
import ml_dtypes
import concourse.bass as bass
import concourse.mybir as mybir
from concourse.bass_utils import run_bass_kernel_spmd

F32 = mybir.dt.float32
BF16 = mybir.dt.bfloat16
ALU = mybir.AluOpType
AF = mybir.ActivationFunctionType
AX = mybir.AxisListType

NCORES = 8
L = 2048
D = 1024
NT = L // 128
KC = D // 128
DEPTH = 2
IN_W = 2340
NEXP = 16
DEXP = 256
NEG = -30000.0
TOPK = 256
NBIS = 10

COMPUTE = ("pe", "act", "dve", "pool")


class Op:
    __slots__ = ("eng", "fn", "deps", "idx", "signal", "dma", "sem", "semval")

    def __init__(self, eng, fn, dma):
        self.eng = eng
        self.fn = fn
        self.deps = set()
        self.signal = False
        self.dma = dma
        self.sem = None
        self.semval = 0


def region(ap):
    t = ap.tensor
    dims = ap.ap
    off = ap.offset
    kind = type(t).__name__
    if kind.startswith("DRam"):
        ext = 1
        for (st, cnt) in dims:
            ext += (cnt - 1) * abs(st)
        return (t.name, off, off + ext, 0, 1)
    row, npart = dims[0]
    esz = mybir.dt.size(ap.dtype)
    if row == 0:
        row = 1 << 40
    p0 = off // row
    col0 = off % row
    ext = 1
    for (st, cnt) in dims[1:]:
        ext += (cnt - 1) * abs(st)
    lo = col0 * esz
    hi = (col0 + ext) * esz
    if kind.startswith("PSum"):
        return (t.name, (lo // 2048) * 2048, ((hi + 2047) // 2048) * 2048, 0, 128)
    return (t.name, lo, hi, p0, p0 + npart)


class Prog:
    def __init__(self):
        self.ops = []
        self.bufs = {}

    def add(self, eng, fn, reads=(), writes=(), dma=False):
        op = Op(eng, fn, dma)
        op.idx = len(self.ops)
        ops = self.ops
        ops.append(op)
        for ap in reads:
            name, lo, hi, p0, p1 = region(ap)
            b = self.bufs.setdefault(name, [[], []])
            for (a, c, q0, q1, w) in b[0]:
                if a < hi and lo < c and q0 < p1 and p0 < q1:
                    op.deps.add(w)
            if name == "ps":
                for (a, c, q0, q1, r) in b[1]:
                    if a < hi and lo < c and ops[r].eng != eng:
                        op.deps.add(r)
            if not dma:
                b[1] = [r for r in b[1] if not (ops[r[4]].eng == eng and not ops[r[4]].dma
                                                and lo <= r[0] and r[1] <= hi and p0 <= r[2] and r[3] <= p1)]
            b[1].append((lo, hi, p0, p1, op.idx))
        for ap in writes:
            name, lo, hi, p0, p1 = region(ap)
            b = self.bufs.setdefault(name, [[], []])
            for (a, c, q0, q1, w) in b[0]:
                if a < hi and lo < c and q0 < p1 and p0 < q1:
                    op.deps.add(w)
            for (a, c, q0, q1, r) in b[1]:
                if a < hi and lo < c and q0 < p1 and p0 < q1:
                    op.deps.add(r)
            b[0] = [w for w in b[0] if not (lo <= w[0] and w[1] <= hi and p0 <= w[2] and w[3] <= p1)]
            b[1] = [r for r in b[1] if not (lo <= r[0] and r[1] <= hi and p0 <= r[2] and r[3] <= p1)]
            b[0].append((lo, hi, p0, p1, op.idx))
        op.deps.discard(op.idx)
        return op

    def emit(self, nc, final_wait_ops=()):
        ops = self.ops
        for op in ops:
            for d in op.deps:
                dop = ops[d]
                if dop.eng == "pe" and op.eng == "pe" and not dop.dma and not op.dma:
                    continue
                dop.signal = True
        for op in final_wait_ops:
            op.signal = True
        for op in ops:
            if op.dma:
                op.signal = True
        engs = {"pe": nc.tensor, "act": nc.scalar, "dve": nc.vector, "pool": nc.gpsimd, "sp": nc.sync}
        with contextlib.ExitStack() as st:
            csem = {e: st.enter_context(nc.semaphore("s_" + e)) for e in COMPUTE}
            ccount = {e: 0 for e in COMPUTE}
            NDMA = 32
            NPOOL = 8
            dsem = [st.enter_context(nc.semaphore("d%d" % i)) for i in range(NDMA)]
            dcount = [0] * NDMA
            dlast = [None] * NDMA
            rr = {"pool": 0, "hw": 0}
            for op in ops:
                if op.dma:
                    if op.signal:
                        if op.eng == "pool":
                            k = rr["pool"] % NPOOL
                            rr["pool"] += 1
                        else:
                            k = NPOOL + rr["hw"] % (NDMA - NPOOL)
                            rr["hw"] += 1
                        if dlast[k] is not None:
                            op.deps.add(dlast[k].idx)
                        dcount[k] += 16
                        op.sem = dsem[k]
                        op.semval = dcount[k]
                        dlast[k] = op
                elif op.signal:
                    ccount[op.eng] += 1
                    op.sem = csem[op.eng]
                    op.semval = ccount[op.eng]
            per_eng = {e: [] for e in engs}
            for op in ops:
                per_eng[op.eng].append(op)
            block = st.enter_context(nc.Block())

            def run_stream(ename, eobj):
                seen = {}
                for op in per_eng[ename]:
                    need = {}
                    for d in op.deps:
                        dop = ops[d]
                        if dop.sem is None:
                            continue
                        if dop.eng == "pe" and ename == "pe" and not dop.dma and not op.dma:
                            continue
                        key = id(dop.sem)
                        if key not in need or need[key][1] < dop.semval:
                            need[key] = (dop.sem, dop.semval)
                    for key, (sem, val) in need.items():
                        if seen.get(key, 0) >= val:
                            continue
                        eobj.wait_ge(sem, val)
                        seen[key] = val
                    ins = op.fn(eobj)
                    if op.sem is not None:
                        ins.then_inc(op.sem, 16 if op.dma else 1)
                if ename == "sp":
                    for k in range(NDMA):
                        if dcount[k] > 0:
                            eobj.wait_ge(dsem[k], dcount[k])

            @block.tensor
            def _(e):
                run_stream("pe", e)

            @block.scalar
            def _(e):
                run_stream("act", e)

            @block.vector
            def _(e):
                run_stream("dve", e)

            @block.gpsimd
            def _(e):
                run_stream("pool", e)

            @block.sync
            def _(e):
                run_stream("sp", e)


CT_ID = 0
CT_TRINEG = 128
CT_TRIT = 256
CT_NEGI = 384
CT_CS64 = 512
CT_CS32 = 768
CT_PADC = 896
CT_NEGM = 1024
CT_POW2 = 1152
CT_OH = 1184
CT_W = 1184 + 2048


def make_ctab():
    t = np.zeros((128, CT_W), np.float32)
    p = np.arange(128)
    t[:, CT_ID:CT_ID + 128] = np.eye(128)
    t[:, CT_TRINEG:CT_TRINEG + 128] = np.where(p[None, :] <= p[:, None], 0.0, -3.0e38)
    t[:, CT_TRIT:CT_TRIT + 128] = np.where(p[:, None] <= p[None, :], 0.0, NEG)
    t[:, CT_NEGI:CT_NEGI + 128] = NEG * np.eye(128)
    pos = (np.arange(NT)[None, :] * 128 + p[:, None]).astype(np.float32)
    for (base, rot) in ((CT_CS64, 16), (CT_CS32, 8)):
        half = rot // 2
        inv = np.exp(np.float32(-math.log(500000.0)) * (np.arange(half, dtype=np.float32) * np.float32(2.0) / np.float32(rot))).astype(np.float32)
        ang = (pos[:, :, None] * inv[None, None, :]).astype(np.float32)
        cs = np.concatenate([np.cos(ang), np.sin(ang)], axis=-1).astype(np.float32)
        t[:, base:base + NT * rot] = cs.reshape(128, NT * rot)
    own = np.arange(NT) // 2
    n = np.arange(8)
    t[:, CT_PADC:CT_PADC + 128] = np.where(n[None, :] < own[:, None], 0.0, -1.0e30).reshape(1, 128)
    t[:, CT_NEGM:CT_NEGM + 128] = np.where(n[None, :] < own[:, None], NEG, 0.0).reshape(1, 128)
    t[:, CT_POW2:CT_POW2 + 32] = (2.0 ** -np.arange(32))[None, :]
    oh = np.zeros((128, 16, 128), np.float32)
    for e in range(16):
        oh[e, e, :] = 1.0
    t[:, CT_OH:CT_OH + 2048] = oh.reshape(128, 2048)
    return t


def make_kind():
    k = np.zeros((8, L), np.float32)
    for n in range(8):
        k[n, n * 256:(n + 1) * 256] = 1.0
    return k


def build_program(dbg=None, layers=DEPTH, stop_after=None):
    dbg = dbg or {}
    nc = bass.Bass("TRN2", target_bir_lowering=False)

    def din(name, shape):
        return nc.dram_tensor(name, list(shape), F32, kind="ExternalInput").ap()

    x_d = din("x", [2, L, D])
    c_d = din("c", [2, D])
    wada_d = din("w_ada", [DEPTH, D, 6 * D])
    bada_d = din("b_ada", [DEPTH, 6 * D])
    gmix_d = din("g_mix", [DEPTH, D])
    win_d = din("w_in", [DEPTH, D, IN_W])
    wdw_d = din("w_dw", [DEPTH, 31, 256])
    bdw_d = din("b_dw", [DEPTH, 256])
    lng_d = din("ln_conv_g", [DEPTH, 256])
    lnb_d = din("ln_conv_b", [DEPTH, 256])
    wo_d = din("w_o", [DEPTH, D, D])
    gffn_d = din("g_ffn", [DEPTH, D])
    wr_d = din("w_router", [D, NEXP])
    br_d = din("b_router", [NEXP])
    wg_d = din("w_gate", [DEPTH, NEXP, D, DEXP])
    wu_d = din("w_up", [DEPTH, NEXP, D, DEXP])
    wd_d = din("w_down", [DEPTH, NEXP, DEXP, D])
    gfin_d = din("g_final", [D])
    ctab_d = din("ctab", [128, CT_W])
    kind_d = din("kind", [8, L])
    out_d = nc.dram_tensor("out", [2, L, D], F32, kind="ExternalOutput").ap()
    xs_d = nc.dram_tensor("xs_scr", [2, L, D], F32).ap()
    mod_d = nc.dram_tensor("mod_scr", [DEPTH, 2, 6 * D], F32).ap()
    dbg_d = {k: nc.dram_tensor("dbg_" + k, list(shp), F32, kind="ExternalOutput").ap() for k, shp in dbg.items()}

    P = Prog()
    finals = []

    with contextlib.ExitStack() as st:
        AW = 53000
        sb = st.enter_context(nc.sbuf_tensor("arena", [128, AW], F32))
        ps = st.enter_context(nc.psum_tensor("ps", [128, 8, 512], F32))

        class Alloc:
            def __init__(self, base, limit):
                self.o = base
                self.limit = limit

            def f32(self, n):
                o = self.o
                self.o += n
                assert self.o <= self.limit, (self.o, self.limit)
                return sb[:, o:o + n]

            def bf(self, n):
                w = (n + 1) // 2
                o = self.o
                self.o += w
                assert self.o <= self.limit, (self.o, self.limit)
                return sb[:, o:o + w].bitcast(BF16)

        def aps(*xs):
            return [x for x in xs if x is not None and not isinstance(x, (int, float))]

        def mm(out, lhsT, rhs, start=True, stop=True):
            P.add("pe", lambda e: e.matmul(out, lhsT=lhsT, rhs=rhs, start=start, stop=stop), reads=[lhsT, rhs], writes=[out])

        def tr(out, in_, ident):
            P.add("pe", lambda e: e.transpose(out=out, in_=in_, identity=ident), reads=[in_, ident], writes=[out])

        def act(out, in_, func, scale=1.0, bias=None, accum=None):
            kw = {}
            if bias is not None:
                kw["bias"] = bias
            if accum is not None:
                kw["accum_out"] = accum
            P.add("act", lambda e: e.activation(out=out, in_=in_, func=func, scale=scale, **kw),
                  reads=aps(in_, scale, bias), writes=aps(out, accum))

        def ts(eng, out, in0, s1, s2=None, op0=ALU.mult, op1=None, accum=None):
            kw = {}
            if op1 is not None:
                kw["op1"] = op1
            if accum is not None:
                kw["accum_out"] = accum
            P.add(eng, lambda e: e.tensor_scalar(out=out, in0=in0, scalar1=s1, scalar2=s2, op0=op0, **kw),
                  reads=aps(in0, s1, s2), writes=aps(out, accum))

        def tt(eng, out, in0, in1, op):
            P.add(eng, lambda e: e.tensor_tensor(out=out, in0=in0, in1=in1, op=op), reads=[in0, in1], writes=[out])

        def stt(out, in0, scalar, in1, op0, op1):
            P.add("dve", lambda e: e.scalar_tensor_tensor(out=out, in0=in0, scalar=scalar, in1=in1, op0=op0, op1=op1),
                  reads=aps(in0, scalar, in1), writes=[out])

        def cp(eng, out, in_):
            if eng == "act":
                P.add("act", lambda e: e.activation(out=out, in_=in_, func=AF.Copy), reads=[in_], writes=[out])
            else:
                P.add(eng, lambda e: e.tensor_copy(out=out, in_=in_), reads=[in_], writes=[out])

        def memset(eng, out, val):
            P.add(eng, lambda e: e.memset(out, val), writes=[out])

        def dma(eng, out, in_):
            return P.add(eng, lambda e: e.dma_start(out=out, in_=in_), reads=[in_], writes=[out], dma=True)

        def red(out, in_, op, axis=AX.X):
            P.add("dve", lambda e: e.tensor_reduce(out=out, in_=in_, axis=axis, op=op), reads=[in_], writes=[out])

        def recip(out, in_):
            P.add("dve", lambda e: e.reciprocal(out=out, in_=in_), reads=[in_], writes=[out])

        def max8(out, in_):
            P.add("dve", lambda e: e.max(out=out, in_=in_), reads=[in_], writes=[out])

        def dump(name, ap_sb, dram_ap=None):
            if name in dbg_d:
                finals.append(dma("sp", dram_ap if dram_ap is not None else dbg_d[name], ap_sb))

        def psb(b, lo=0, hi=512):
            return ps[:, b, lo:hi]

        def psbf(b):
            return ps[:, b, :].bitcast(BF16)

        A = Alloc(0, AW)
        ctab = A.f32(CT_OH)
        ident_f = ctab[:, CT_ID:CT_ID + 128]
        trineg = ctab[:, CT_TRINEG:CT_TRINEG + 128]
        cs64 = ctab[:, CT_CS64:CT_CS64 + 256].rearrange("p (i r) -> p i r", r=16)
        cs32 = ctab[:, CT_CS32:CT_CS32 + 128].rearrange("p (i r) -> p i r", r=8)
        padc = ctab[:, CT_PADC:CT_PADC + 128].rearrange("p (i n) -> p i n", n=8)
        negm = ctab[:, CT_NEGM:CT_NEGM + 128].rearrange("p (i n) -> p i n", n=8)
        pow2 = ctab[:, CT_POW2:CT_POW2 + 32]
        ident_b = A.bf(128)
        trit_b = A.bf(128)
        negi3 = A.bf(384).rearrange("p (g q) -> p g q", g=3)
        onesq_b = A.bf(128)
        ones_b = A.bf(128)
        oh_b = A.bf(2048).rearrange("p (e m) -> p e m", e=16)
        cst = A.f32(8)
        rows = A.f32(128)
        vecT = A.f32(128)
        gm = A.f32(16)
        brt_bc = A.f32(16)
        wrT = A.f32(128)
        wr_hi = A.bf(128)
        wr_lo = A.bf(128)
        brow = A.f32(16)
        brow_hi = A.bf(16)
        brow_lo = A.bf(16)
        cactT = A.bf(16)
        sh2col = A.bf(8)
        hT = A.bf(KC * L).rearrange("p (k t) -> p k t", k=KC)
        yT = A.bf(KC * L).rearrange("p (k t) -> p k t", k=KC)
        S0 = A.o

        dma("sp", ctab, ctab_d[:, 0:CT_OH])
        cp("pool", ident_b, ident_f)
        cp("pool", trit_b, ctab[:, CT_TRIT:CT_TRIT + 128])
        for g in range(3):
            cp("pool", negi3[:, g, :], ctab[:, CT_NEGI:CT_NEGI + 128])
        memset("pool", onesq_b, 1.0 / 256.0)
        memset("pool", ones_b, 1.0)
        ohf = sb[:, S0:S0 + 2048]
        dma("sp", ohf, ctab_d[:, CT_OH:CT_OH + 2048])
        cp("pool", oh_b.rearrange("p e m -> p (e m)"), ohf)
        memset("pool", cst[:, 0:1], 1e-6)
        memset("pool", rows, 0.0)
        dma("sp", brt_bc, br_d.partition_broadcast(128))
        for kc in range(KC):
            dma("sp", wrT[:, kc * 16:(kc + 1) * 16], wr_d[kc * 128:(kc + 1) * 128, :])

        PA = Alloc(S0 + 2048, AW)
        cin = PA.f32(128)
        dma("sp", cin[0:16, :], c_d.rearrange("b (k p) -> (b k) p", p=128))
        act(cin[0:16, :], cin[0:16, :], AF.Silu)
        tr(psb(0, 0, 16), cin[0:16, :], ident_f[0:16, 0:16])
        cp("dve", cactT, psb(0, 0, 16))
        cactT3 = cactT.rearrange("p (b k) -> p b k", b=2)
        cbc = PA.bf(KC * 128).rearrange("p (k m) -> p k m", k=KC)
        for b in range(2):
            cp("dve", cbc[:, :, b * 64:(b + 1) * 64], cactT3[:, b, :].unsqueeze(2).to_broadcast([128, KC, 64]))
        wada_b = PA.bf(KC * 1536).rearrange("p (k n) -> p k n", k=KC)
        bada = PA.f32(6 * D)
        modrow = PA.f32(6 * D)
        import os
        PRO = os.environ.get("PRO", "all")
        for l in range(DEPTH if PRO != "nowada" else 0):
            dma("sp", bada, bada_d[l].partition_broadcast(128))
            for q in range(4):
                for kc in range(KC):
                    dma("pool", wada_b[:, kc, :], wada_d[l, kc * 128:(kc + 1) * 128, q * 1536:(q + 1) * 1536])
                for j in range(3):
                    bank = (q * 3 + j) % 2
                    c0 = q * 1536 + j * 512
                    for kc in range(KC):
                        mm(psb(bank), cbc[:, kc, :], wada_b[:, kc, j * 512:(j + 1) * 512], start=(kc == 0), stop=(kc == KC - 1))
                    tt("dve", modrow[:, c0:c0 + 512], psb(bank), bada[:, c0:c0 + 512], ALU.add)
            dma("sp", mod_d[l, 0:1, :], modrow[0:1, :])
            dma("sp", mod_d[l, 1:2, :], modrow[64:65, :])
            if l == 0 and "mod0" in dbg_d:
                finals.append(dma("sp", dbg_d["mod0"][0:1, :], modrow[0:1, :]))
                finals.append(dma("sp", dbg_d["mod0"][1:2, :], modrow[64:65, :]))

        def load_vectors(l, s):
            srcs = [mod_d[l, s, 0:D], mod_d[l, s, D:2 * D], mod_d[l, s, 3 * D:4 * D], mod_d[l, s, 4 * D:5 * D],
                    gmix_d[l], gffn_d[l]]
            for j, src in enumerate(srcs):
                dma("sp", rows[8 * j:8 * j + 8, :], src.rearrange("(c p) -> c p", p=128))
            dma("sp", rows[48:79, :], wdw_d[l, :, 0:128])
            dma("sp", rows[79:110, :], wdw_d[l, :, 128:256])
            dma("sp", rows[110:112, :], bdw_d[l].rearrange("(c p) -> c p", p=128))
            dma("sp", rows[112:114, :], lng_d[l].rearrange("(c p) -> c p", p=128))
            dma("sp", rows[114:116, :], lnb_d[l].rearrange("(c p) -> c p", p=128))
            tr(psb(7, 0, 128), rows, ident_f)
            cp("dve", vecT, psb(7, 0, 128))
            stt(gm[:, 0:8], vecT[:, 8:16], 1.0, vecT[:, 32:40], ALU.add, ALU.mult)
            stt(gm[:, 8:16], vecT[:, 24:32], 1.0, vecT[:, 40:48], ALU.add, ALU.mult)

        sh1 = vecT[:, 0:8]
        sh2 = vecT[:, 16:24]
        wdwT = [vecT[:, 48:79], vecT[:, 79:110]]
        bdwT = vecT[:, 110:112]
        lngT = vecT[:, 112:114]
        lnbT = vecT[:, 114:116]

        def rms_rstd(xt, junk, ssum, rstd):
            act(junk, xt, AF.Square, accum=ssum)
            act(ssum, ssum, AF.Sqrt, scale=1.0 / D, bias=cst[:, 0:1])
            recip(rstd, ssum)

        def phase1(l, s, SA):
            xsrc = x_d if l == 0 else xs_d
            xt = [SA.f32(D) for _ in range(2)]
            xn = [SA.bf(D) for _ in range(2)]
            junk = SA.bf(D)
            st2 = SA.f32(4)
            def p1(i):
                x_t = xt[i % 2]
                ssum = st2[:, (i % 2) * 2:(i % 2) * 2 + 1]
                rstd = st2[:, (i % 2) * 2 + 1:(i % 2) * 2 + 2]

                def fa():
                    dma("sp", x_t, xsrc[s, i * 128:(i + 1) * 128, :])
                    rms_rstd(x_t, junk, ssum, rstd)
                    act(xn[i % 2], x_t, AF.Identity, scale=rstd)

                def fb():
                    pb = psbf(i % 2).rearrange("p (k t) -> p k t", k=KC)
                    for kc in range(KC):
                        tr(pb[:, kc, :], xn[i % 2][:, kc * 128:(kc + 1) * 128], ident_b)
                    for kc in range(KC):
                        if i % 2 == 0:
                            act(hT[:, kc, i * 128:(i + 1) * 128], pb[:, kc, :], AF.Identity, scale=gm[:, kc:kc + 1], bias=sh1[:, kc:kc + 1])
                        else:
                            ts("dve", hT[:, kc, i * 128:(i + 1) * 128], pb[:, kc, :], gm[:, kc:kc + 1], sh1[:, kc:kc + 1], ALU.mult, ALU.add)
                return (fa, fb)
            for f in pipelined([p1(i) for i in range(NT)], 1):
                f()

        def load_win(l, c0, c1, SA):
            w = SA.bf(KC * (c1 - c0)).rearrange("p (k n) -> p k n", k=KC)
            for kc in range(KC):
                dma("pool", w[:, kc, :], win_d[l, kc * 128:(kc + 1) * 128, c0:c1])
            return w

        def phase2(l, s, SA):
            w = load_win(l, 0, 512, SA)
            apad = SA.bf(2 * (L + 32)).rearrange("p (c t) -> p c t", c=2)
            dg = SA.bf(2 * 31 * 128).rearrange("p (c j m) -> p c j m", c=2, j=31)
            acc = SA.f32(2 * L).rearrange("p (c t) -> p c t", c=2)
            sg = [SA.f32(512) for _ in range(2)]
            ybf = SA.bf(2 * 512).rearrange("p (c t) -> p c t", c=2)
            ysq = SA.bf(2 * 512).rearrange("p (c t) -> p c t", c=2)
            tmp = SA.f32(512)
            rstd = SA.f32(512)
            dd = [SA.f32(512) for _ in range(2)]
            memset("pool", apad[:, :, 0:32], 0.0)
            for cc in range(2):
                for j in range(31):
                    ts("dve" if j % 2 == 0 else "pool", dg[:, cc, j, :], ident_f, wdwT[cc][:, j:j + 1], 1.0, ALU.mult, ALU.mult)
            for tg in range(4):
                tsl = slice(tg * 512, (tg + 1) * 512)
                for cc in range(2):
                    for half, col in ((0, cc * 128), (1, 256 + cc * 128)):
                        bank = cc * 2 + half
                        for kc in range(KC):
                            mm(psb(bank), w[:, kc, col:col + 128], hT[:, kc, tsl], start=(kc == 0), stop=(kc == KC - 1))
                    act(sg[cc], psb(cc * 2 + 1), AF.Sigmoid)
                    tt("dve", apad[:, cc, 32 + tg * 512:32 + (tg + 1) * 512], psb(cc * 2), sg[cc], ALU.mult)
            for tg in range(4):
                tsl = slice(tg * 512, (tg + 1) * 512)
                for cc in range(2):
                    for j in range(31):
                        o = 2 + j + tg * 512
                        mm(psb(4 + cc), dg[:, cc, j, :], apad[:, cc, o:o + 512], start=(j == 0), stop=(j == 30))
                    act(acc[:, cc, tsl], psb(4 + cc), AF.Identity, bias=bdwT[:, cc:cc + 1])
                    cp("dve", ybf[:, cc, :], acc[:, cc, tsl])
                    act(ysq[:, cc, :], acc[:, cc, tsl], AF.Square)
                for cc in range(2):
                    mm(psb(6), onesq_b, ybf[:, cc, :], start=(cc == 0), stop=(cc == 1))
                for cc in range(2):
                    mm(psb(7), onesq_b, ysq[:, cc, :], start=(cc == 0), stop=(cc == 1))
                cp("act", tmp, psb(6))
                tt("dve", rstd, tmp, tmp, ALU.mult)
                tt("dve", rstd, psb(7), rstd, ALU.subtract)
                ts("dve", rstd, rstd, 0.0, None, ALU.max)
                act(rstd, rstd, AF.Sqrt, bias=cst[:, 0:1])
                recip(rstd, rstd)
                for cc in range(2):
                    tt("dve", dd[cc], acc[:, cc, tsl], tmp, ALU.subtract)
                    tt("dve", dd[cc], dd[cc], rstd, ALU.mult)
                    act(yT[:, cc, tsl], dd[cc], AF.Silu, scale=lngT[:, cc:cc + 1], bias=lnbT[:, cc:cc + 1])

        def rope(dst, src, nh, hd, half, cs_i, tmp):
            cos = cs_i[:, 0:half].unsqueeze(1).to_broadcast([128, nh, half])
            sin = cs_i[:, half:2 * half].unsqueeze(1).to_broadcast([128, nh, half])
            x1 = src[:, :, 0:half]
            x2 = src[:, :, half:2 * half]
            t1 = tmp[:, 0:nh * half].rearrange("p (h r) -> p h r", h=nh)
            t2 = tmp[:, 64:64 + nh * half].rearrange("p (h r) -> p h r", h=nh)
            t3 = tmp[:, 128:128 + nh * half].rearrange("p (h r) -> p h r", h=nh)
            t4 = tmp[:, 192:192 + nh * half].rearrange("p (h r) -> p h r", h=nh)
            tt("dve", t1, x1, cos, ALU.mult)
            tt("dve", t2, x2, sin, ALU.mult)
            tt("dve", t3, x2, cos, ALU.mult)
            tt("dve", t4, x1, sin, ALU.mult)
            tt("dve", dst[:, :, 0:half], t1, t2, ALU.subtract)
            tt("dve", dst[:, :, half:2 * half], t3, t4, ALU.add)
            cp("act", dst[:, :, 2 * half:hd], src[:, :, 2 * half:hd])

        def pipelined(items, depth):
            out = []
            n = len(items)
            for k in range(n + depth):
                def f(k=k):
                    if k < n and items[k][0] is not None:
                        items[k][0]()
                    if k - depth >= 0 and items[k - depth][1] is not None:
                        items[k - depth][1]()
                out.append(f)
            return out

        def phase3(l, s, SA):
            qT = SA.bf(6 * L).rearrange("p (h t) -> p h t", h=6)
            kT = SA.bf(L)
            vd = SA.bf(NT * 128).rearrange("p (c m) -> p c m", c=NT)
            vds = SA.bf(NT * 128).rearrange("p (c m) -> p c m", c=NT)
            iqT = SA.bf(4 * L).rearrange("p (h t) -> p h t", h=4)
            ikT = SA.bf(L)
            widx = SA.f32(NT * 4).rearrange("p (i h) -> p i h", h=4)
            mark = SA.o
            w = load_win(l, 512, 1188, SA)
            qs = [SA.bf(7 * 64).rearrange("p (h d) -> p h d", h=7) for _ in range(2)]
            iqs = [SA.bf(5 * 32).rearrange("p (h d) -> p h d", h=5) for _ in range(2)]
            rtmp = [SA.f32(256) for _ in range(2)]
            rtmp2 = [SA.f32(256) for _ in range(2)]
            memset("pool", vd[:, :, 64:128], 1.0)
            memset("pool", vds[:, :, 0:64], 1.0)
            slot = {0: 0, 2: 1, 4: 2, 1: 3, 3: 4, 5: 5}
            def p3a(i):
                tsl = slice(i * 128, (i + 1) * 128)
                b0 = (i % 2) * 2

                def fa():
                    for (bank, c0, c1) in ((b0, 0, 512), (b0 + 1, 512, 676)):
                        for kc in range(KC):
                            mm(psb(bank, 0, c1 - c0), hT[:, kc, tsl], w[:, kc, c0:c1], start=(kc == 0), stop=(kc == KC - 1))
                    pq = psb(b0, 0, 448).rearrange("p (h d) -> p h d", h=7)
                    rope(qs[i % 2], pq, 7, 64, 8, cs64[:, i, :], rtmp[i % 2])
                    cp("act", vd[:, i, 0:64], psb(b0, 448, 512))
                    cp("dve", vds[:, i, 64:128], psb(b0, 448, 512))
                    piq = psb(b0 + 1, 0, 160).rearrange("p (h d) -> p h d", h=5)
                    rope(iqs[i % 2], piq, 5, 32, 4, cs32[:, i, :], rtmp2[i % 2])
                    cp("act", widx[:, i, :], psb(b0 + 1, 160, 164))

                def fb():
                    pt = psbf(4 + (i % 2))
                    ptq = pt[:, 0:768].rearrange("p (h t) -> p h t", h=6)
                    for h in range(6):
                        tr(ptq[0:64, slot[h], :], qs[i % 2][:, h, :], ident_b)
                    tr(pt[0:64, 768:896], qs[i % 2][:, 6, :], ident_b)
                    cp("act", qT[0:64, :, tsl], ptq[0:64, :, :])
                    cp("dve", kT[0:64, tsl], pt[0:64, 768:896])
                    pt2 = psbf(6 + (i % 2))
                    pti = pt2[:, 0:512].rearrange("p (h t) -> p h t", h=4)
                    for h in range(4):
                        tr(pti[0:32, h, :], iqs[i % 2][:, h, :], ident_b)
                    tr(pt2[0:32, 512:640], iqs[i % 2][:, 4, :], ident_b)
                    cp("act", iqT[0:32, :, tsl], pti[0:32, :, :])
                    cp("dve", ikT[0:32, tsl], pt2[0:32, 512:640])
                return (fa, fb)
            for f in pipelined([p3a(i) for i in range(NT)], 1):
                f()
            SA.o = mark
            score = [SA.f32(L) for _ in range(4)]
            rel = [SA.f32(512) for _ in range(2)]
            junk = [SA.bf(L) for _ in range(2)]
            notsel = [SA.bf(L) for _ in range(4)]
            PT = [SA.bf(768).rearrange("p (g q) -> p g q", g=2) for _ in range(3)]
            bst = [SA.f32(64) for _ in range(4)]
            rec = SA.f32(384)
            selT = [SA.bf(128) for _ in range(3)]
            otS1 = SA.f32(768).rearrange("p (g q) -> p g q", g=2)
            otS = [otS1, otS1]
            cnt_ = {"npt": 0, "lb": 0}

            def score_chunks(i):
                S = 128 * (i + 1)
                sc = score[i % 4]
                tsl = slice(i * 128, (i + 1) * 128)
                nchunk = (S + 511) // 512
                out = []

                def mk(ch):
                    def f():
                        k0 = ch * 512
                        k1 = min(S, k0 + 512)
                        for h in range(4):
                            bank = cnt_["lb"] % 2
                            cnt_["lb"] += 1
                            mm(psb(bank, 0, k1 - k0), iqT[0:32, h, tsl], ikT[0:32, k0:k1])
                            if h == 0:
                                ts("dve", sc[:, k0:k1], psb(bank, 0, k1 - k0), 0.0, widx[:, i, 0:1], ALU.max, ALU.mult)
                            else:
                                r = rel[h % 2]
                                act(r[:, 0:k1 - k0], psb(bank, 0, k1 - k0), AF.Relu)
                                stt(sc[:, k0:k1], r[:, 0:k1 - k0], widx[:, i, h:h + 1], sc[:, k0:k1], ALU.mult, ALU.add)
                        if ch == nchunk - 1:
                            tt("dve", sc[:, i * 128:S], sc[:, i * 128:S], trineg, ALU.add)
                    return f
                for ch in range(nchunk):
                    out.append(mk(ch))
                return out

            def bisect_init(i):
                def f():
                    sc = score[i % 4]
                    bs = bst[i % 4]
                    pre = sc[:, 0:i * 128]
                    mx = bs[:, 0:1]
                    mn = bs[:, 1:2]
                    cand = bs[:, 2:3]
                    steps = bs[:, 8:8 + NBIS + 1]
                    red(mx, pre, ALU.max)
                    red(mn, pre, ALU.min)
                    tt("dve", mx, mx, mn, ALU.subtract)
                    ts("dve", steps, pow2[:, 1:NBIS + 2], mx, None, ALU.mult)
                    tt("dve", cand, mn, steps[:, 0:1], ALU.add)
                return f

            def bisect_steps(i, engs):
                S = 128 * (i + 1)
                sc = score[i % 4]
                bs = bst[i % 4]
                cand = bs[:, 2:3]
                cnt = bs[:, 3:4]
                dlt = bs[:, 4:5]
                steps = bs[:, 8:8 + NBIS + 1]
                out = []

                def mk(it):
                    def f():
                        if engs[it] == "dve":
                            ts("dve", junk[0][:, 0:S], sc[:, 0:S], cand, None, ALU.is_ge, ALU.add, accum=cnt)
                            ts("dve", dlt, cnt, TOPK - 0.5, 0.5, ALU.is_ge, ALU.subtract)
                        else:
                            act(junk[1][:, 0:S], sc[:, 0:S], AF.Sign, scale=-1.0, bias=cand, accum=cnt)
                            ts("dve", dlt, cnt, S - 2 * TOPK + 1.0, 0.5, ALU.is_le, ALU.subtract)
                        if it == NBIS - 1:
                            ts("dve", dlt, dlt, 0.5, None, ALU.subtract)
                        stt(cand, dlt, steps[:, it:it + 1], cand, ALU.mult, ALU.add)
                    return f
                for it in range(NBIS):
                    out.append(mk(it))
                return out

            def finish(i):
                S = 128 * (i + 1)
                sc = score[i % 4]
                ns = notsel[i % 4]
                if i >= 2:
                    ts("dve", ns[:, 0:S], sc[:, 0:S], bst[i % 4][:, 2:3], None, ALU.is_ge)
                else:
                    ts("dve", ns[:, 0:S], sc[:, 0:S], -1.0e37, None, ALU.is_ge)

            def attention(i):
                tsl = slice(i * 128, (i + 1) * 128)
                ns = notsel[i % 4]
                items = []

                def mk(c):
                    st_ = {}

                    def fa():
                        csl = slice(c * 128, (c + 1) * 128)
                        st_["sb0"] = 2 + 2 * (cnt_["npt"] % 2)
                        st_["pt"] = PT[cnt_["npt"] % 3]
                        st_["selT"] = selT[cnt_["npt"] % 3]
                        cnt_["npt"] += 1
                        for g in range(2):
                            mm(psb(st_["sb0"] + g, 0, 384), kT[0:64, csl], qT[0:64, 3 * g:3 * g + 3, tsl], start=True, stop=True)
                        tp = psb(st_["sb0"], 384, 448).bitcast(BF16)
                        tr(tp, ns[:, csl], ident_b)
                        cp("dve", st_["selT"], tp)

                    def fb():
                        act(st_["pt"], ps[:, st_["sb0"]:st_["sb0"] + 2, 0:384], AF.Exp, scale=0.125)
                        for g in range(2):
                            ptg = st_["pt"][:, g, :].rearrange("p (h q) -> p h q", h=3)
                            tt("dve", ptg, ptg, st_["selT"].unsqueeze(1).to_broadcast([128, 3, 128]), ALU.mult)
                        for g in range(2):
                            mm(psb(6 + g, 0, 384), (vd if g == 0 else vds)[:, c, :], st_["pt"][:, g, :], start=(c == 0), stop=(c == i))
                    return (fa, fb)

                def norm():
                    o_ = otS[i % 2]
                    cp("dve", o_, ps[:, 6:8, 0:384])
                    act(rec[0:64, :], o_[64:128, 0, :], AF.Ln)
                    act(rec[0:64, :], rec[0:64, :], AF.Exp, scale=-1.0)
                    tt("dve", yT[0:64, 2:5, tsl], o_[0:64, 0, :].rearrange("p (h q) -> p h q", h=3), rec[0:64, :].rearrange("p (h q) -> p h q", h=3), ALU.mult)
                    act(rec[64:128, :], o_[0:64, 1, :], AF.Ln)
                    act(rec[64:128, :], rec[64:128, :], AF.Exp, scale=-1.0)
                    tt("dve", yT[64:128, 2:5, tsl], o_[64:128, 1, :].rearrange("p (h q) -> p h q", h=3), rec[64:128, :].rearrange("p (h q) -> p h q", h=3), ALU.mult)
                for c in range(i + 1):
                    items.append(mk(c))
                items.append((None, norm))
                return items

            for f in score_chunks(0) + score_chunks(1):
                f()
            pending = []
            for j in range(NT // 2):
                ta, tb = 2 * j, 2 * j + 1
                sa = bisect_steps(ta, ["act" if it % 2 == 1 else "dve" for it in range(NBIS)]) if ta >= 2 else []
                sb_ = bisect_steps(tb, ["act"] * NBIS) if tb >= 2 else []
                nxt = []
                if j + 1 < NT // 2:
                    nxt = score_chunks(ta + 2) + score_chunks(tb + 2) + [bisect_init(ta + 2), bisect_init(tb + 2)]
                nr = NBIS if sa else 1
                for it in range(nr):
                    if sb_:
                        sb_[it]()
                    if sa:
                        sa[it]()
                    left = nr - it
                    for _ in range((len(pending) + left - 1) // left):
                        pending.pop(0)()
                    for _ in range((len(nxt) + left - 1) // left):
                        nxt.pop(0)()
                while pending:
                    pending.pop(0)()
                while nxt:
                    nxt.pop(0)()
                finish(ta)
                finish(tb)
                pending = pipelined(attention(ta) + attention(tb), 1)
            while pending:
                pending.pop(0)()

        def phase4(l, s, SA):
            w = load_win(l, 1188, 2340, SA)
            qa = SA.bf(6 * L).rearrange("p (h t) -> p h t", h=6)
            ka = SA.bf(6 * L).rearrange("p (h t) -> p h t", h=6)
            vm = SA.bf(NT * 6 * 128).rearrange("p (c h m) -> p c h m", c=NT, h=6)
            kmT = SA.bf(6 * 8).rearrange("p (h n) -> p h n", h=6)
            kms = SA.f32(6 * 8).rearrange("p (h n) -> p h n", h=6)
            qs = [SA.bf(6 * 64).rearrange("p (h d) -> p h d", h=6) for _ in range(2)]
            ks = [SA.bf(6 * 64).rearrange("p (h d) -> p h d", h=6) for _ in range(2)]
            qb = [SA.bf(6 * 72).rearrange("p (h d) -> p h d", h=6) for _ in range(2)]
            rtmp = [SA.f32(256) for _ in range(2)]
            rtmp2 = [SA.f32(256) for _ in range(2)]
            gmk = [SA.f32(48).rearrange("p (h n) -> p h n", h=6) for _ in range(2)]
            g8 = [SA.f32(48).rearrange("p (h n) -> p h n", h=6) for _ in range(2)]
            kindf = SA.f32(L)
            dma("sp", kindf[64:72, :], kind_d)
            for h in range(6):
                cp("pool", ka[64:72, h, :], kindf[64:72, :])
            for par in range(2):
                memset("pool", qb[par], 0.0)
            for h in range(6):
                if h % 2 == 0:
                    memset("pool", vm[:, :, h, 64:128], 1.0)
                else:
                    memset("pool", vm[:, :, h, 0:64], 1.0)
            def p4a(i):
                tsl = slice(i * 128, (i + 1) * 128)
                b0 = (i % 2) * 3

                def fa():
                    for j in range(3):
                        for kc in range(KC):
                            mm(psb(b0 + j, 0, 384), hT[:, kc, tsl], w[:, kc, j * 384:(j + 1) * 384], start=(kc == 0), stop=(kc == KC - 1))
                    pq = psb(b0, 0, 384).rearrange("p (h d) -> p h d", h=6)
                    pk = psb(b0 + 1, 0, 384).rearrange("p (h d) -> p h d", h=6)
                    pv = psb(b0 + 2, 0, 384).rearrange("p (h d) -> p h d", h=6)
                    rope(qs[i % 2], pq, 6, 64, 8, cs64[:, i, :], rtmp[i % 2])
                    rope(ks[i % 2], pk, 6, 64, 8, cs64[:, i, :], rtmp2[i % 2])
                    for h in range(6):
                        o = 0 if h % 2 == 0 else 64
                        cp("act" if h % 2 == 0 else "dve", vm[:, i, h, o:o + 64], pv[:, h, :])

                def fb():
                    ptq = psbf(6)[:, 0:768].rearrange("p (h t) -> p h t", h=6)
                    ptk = psbf(7)[:, 0:768].rearrange("p (h t) -> p h t", h=6)
                    for h in range(6):
                        tr(ptq[0:64, h, :], qs[i % 2][:, h, :], ident_b)
                    for h in range(6):
                        tr(ptk[0:64, h, :], ks[i % 2][:, h, :], ident_b)
                    cp("act", qa[0:64, :, tsl], ptq[0:64, :, :])
                    cp("dve", ka[0:64, :, tsl], ptk[0:64, :, :])
                return (fa, fb)
            for f in pipelined([p4a(i) for i in range(NT)], 1):
                f()
            for h in range(6):
                red(kms[0:64, h, :], ka[0:64, h, :].rearrange("p (n k) -> p n k", n=8), ALU.add)
            ts("dve", kmT[0:64, :, :], kms[0:64, :, :], 1.0 / 256.0, None, ALU.mult)
            def p4g(i):
                tsl = slice(i * 128, (i + 1) * 128)

                def fa():
                    pg = psb(i % 2, 0, 48).rearrange("p (h n) -> p h n", h=6)
                    for h in range(6):
                        mm(pg[:, h, :], qa[0:64, h, tsl], kmT[0:64, h, :])
                    g_ = gmk[i % 2]
                    tt("dve", g_, pg, padc[:, i, :].unsqueeze(1).to_broadcast([128, 6, 8]), ALU.add)
                    for h in range(6):
                        max8(g8[i % 2][:, h, :], g_[:, h, :])
                    tt("dve", g_, g_, g8[i % 2][:, :, 2:3].to_broadcast([128, 6, 8]), ALU.is_lt)
                    tt("dve", qb[i % 2][:, :, 64:72], g_, negm[:, i, :].unsqueeze(1).to_broadcast([128, 6, 8]), ALU.mult)

                def fb():
                    pb = psbf(2 + (i % 2))[:, 0:768].rearrange("p (h t) -> p h t", h=6)
                    for h in range(6):
                        tr(pb[0:72, h, :], qb[i % 2][:, h, :], ident_b)
                    cp("act", qa[64:72, :, tsl], pb[64:72, :, :])
                return (fa, fb)
            for f in pipelined([p4g(i) for i in range(NT)], 1):
                f()
            PT = [SA.bf(512) for _ in range(4)]
            rec = [SA.f32(512) for _ in range(2)]
            st4 = {"npt": 0, "nacc": 0}
            items = []
            for h in range(6):
                odd = h % 2
                for qg in range(4):
                    nch = 4 * qg + 4
                    grp = {}

                    def mk(h, qg, c, nch, grp):
                        st_ = {}

                        def fa():
                            if c == 0:
                                grp["ob"] = 4 + (st4["nacc"] % 2)
                                st4["nacc"] += 1
                            csl = slice(c * 128, (c + 1) * 128)
                            col0 = 0 if c < 4 * qg else 128 * (c - 4 * qg)
                            st_["col0"] = col0
                            st_["sbk"] = st4["npt"] % 4
                            st_["pt"] = PT[st4["npt"] % 4]
                            st4["npt"] += 1
                            diag = c >= 4 * qg
                            mm(psb(st_["sbk"], col0, 512), ka[0:72, h, csl], qa[0:72, h, qg * 512 + col0:(qg + 1) * 512], start=True, stop=not diag)
                            if diag:
                                mm(psb(st_["sbk"], col0, col0 + 128), ident_b, trit_b, start=False, stop=True)

                        def fb():
                            col0 = st_["col0"]
                            act(st_["pt"][:, col0:512], psb(st_["sbk"], col0, 512), AF.Exp, scale=0.125)
                            mm(psb(grp["ob"], col0, 512), vm[:, c, h, :], st_["pt"][:, col0:512], start=(c == 0), stop=(c == nch - 1))
                        return (fa, fb)

                    def mknorm(h, qg, grp, odd):
                        def fn():
                            ob = grp["ob"]
                            qsl = slice(qg * 512, (qg + 1) * 512)
                            r_ = rec[ob % 2]
                            if not odd:
                                recip(r_[0:64, :], psb(ob)[64:128, :])
                                tt("dve", yT[0:64, 5 + h // 2, qsl], psb(ob)[0:64, :], r_[0:64, :], ALU.mult)
                            else:
                                recip(r_[64:128, :], psb(ob)[0:64, :])
                                tt("dve", yT[64:128, 5 + h // 2, qsl], psb(ob)[64:128, :], r_[64:128, :], ALU.mult)
                        return (None, fn)
                    for c in range(nch):
                        items.append(mk(h, qg, c, nch, grp))
                    items.append(mknorm(h, qg, grp, odd))
            for f in pipelined(items, 2):
                f()

        def phase5(l, s, SA, xseq, gates, g1bc):
            xsrc = x_d if l == 0 else xs_d
            wo_b = SA.bf(KC * D).rearrange("p (k n) -> p k n", k=KC)
            stg = [SA.f32(D) for _ in range(2)]
            for kc in range(KC):
                dma("sp", stg[kc % 2], wo_d[l, kc * 128:(kc + 1) * 128, :])
                tt("dve", wo_b[:, kc, :], stg[kc % 2], g1bc, ALU.mult)
            xt = [SA.f32(D) for _ in range(2)]
            xhi = [SA.bf(D) for _ in range(2)]
            xlo = [SA.bf(D) for _ in range(2)]
            xTh = [SA.bf(D).rearrange("p (k t) -> p k t", k=KC) for _ in range(2)]
            xTl = [SA.bf(D).rearrange("p (k t) -> p k t", k=KC) for _ in range(2)]
            junk = SA.bf(D)
            st2 = SA.f32(4)
            wr3 = wrT.rearrange("p (k e) -> p k e", e=16)
            wrm = SA.f32(128).rearrange("p (k e) -> p k e", e=16)
            wrh3 = wr_hi.rearrange("p (k e) -> p k e", e=16)
            wrl3 = wr_lo.rearrange("p (k e) -> p k e", e=16)
            tt("dve", wrm, wr3, gm[:, 8:16].unsqueeze(2).to_broadcast([128, KC, 16]), ALU.mult)
            cp("dve", wrh3, wrm)
            tt("dve", wrm, wrm, wrh3, ALU.subtract)
            cp("dve", wrl3, wrm)
            for kc in range(KC):
                mm(psb(7, 0, 16)[0:1, :], sh2[:, kc:kc + 1], wr3[:, kc, :], start=(kc == 0), stop=(kc == KC - 1))
            cp("dve", brow[0:1, :], psb(7, 0, 16)[0:1, :])
            cp("dve", brow_hi[0:1, :], brow[0:1, :])
            tt("dve", brow[0:1, :], brow[0:1, :], brow_hi[0:1, :], ALU.subtract)
            cp("dve", brow_lo[0:1, :], brow[0:1, :])
            plog = psb(6, 0, NT * 16).rearrange("p (i e) -> p i e", e=16)

            def p5(i):
                tsl = slice(i * 128, (i + 1) * 128)
                b0 = (i % 2) * 2
                x_t = xt[i % 2]
                xs_i = xseq[:, i, :]
                ssum = st2[:, (i % 2) * 2:(i % 2) * 2 + 1]
                rstd = st2[:, (i % 2) * 2 + 1:(i % 2) * 2 + 2]
                pbh = psbf(4).rearrange("p (k t) -> p k t", k=KC)
                pbl = psbf(5).rearrange("p (k t) -> p k t", k=KC)

                def s1():
                    for nh in range(2):
                        for kc in range(KC):
                            mm(psb(b0 + nh), yT[:, kc, tsl], wo_b[:, kc, nh * 512:(nh + 1) * 512], start=(kc == 0), stop=(kc == KC - 1))
                    dma("sp", x_t, xsrc[s, i * 128:(i + 1) * 128, :])
                    for nh in range(2):
                        tt("dve", xs_i[:, nh * 512:(nh + 1) * 512], psb(b0 + nh), x_t[:, nh * 512:(nh + 1) * 512], ALU.add)

                def s2():
                    rms_rstd(xs_i, junk, ssum, rstd)
                    act(xhi[i % 2], xs_i, AF.Identity, scale=rstd)
                    stt(xlo[i % 2], xs_i, rstd, xhi[i % 2], ALU.mult, ALU.subtract)

                def s3():
                    for kc in range(KC):
                        tr(pbh[:, kc, :], xhi[i % 2][:, kc * 128:(kc + 1) * 128], ident_b)
                    for kc in range(KC):
                        tr(pbl[:, kc, :], xlo[i % 2][:, kc * 128:(kc + 1) * 128], ident_b)

                def s4():
                    e1 = "act" if i % 2 == 0 else "dve"
                    e2 = "dve" if i % 2 == 0 else "act"
                    cp(e1, xTh[i % 2], pbh)
                    cp(e2, xTl[i % 2], pbl)
                    for kc in range(KC):
                        if i % 2 == 0:
                            act(hT[:, kc, tsl], pbh[:, kc, :], AF.Identity, scale=gm[:, 8 + kc:9 + kc], bias=sh2[:, kc:kc + 1])
                        else:
                            ts("dve", hT[:, kc, tsl], pbh[:, kc, :], gm[:, 8 + kc:9 + kc], sh2[:, kc:kc + 1], ALU.mult, ALU.add)

                def s5():
                    for kc in range(KC):
                        mm(plog[:, i, :], xTh[i % 2][:, kc, :], wrh3[:, kc, :], start=(kc == 0), stop=False)
                        mm(plog[:, i, :], xTl[i % 2][:, kc, :], wrh3[:, kc, :], start=False, stop=False)
                        mm(plog[:, i, :], xTh[i % 2][:, kc, :], wrl3[:, kc, :], start=False, stop=False)
                    mm(plog[:, i, :], ones_b[0:1, :], brow_hi[0:1, :], start=False, stop=False)
                    mm(plog[:, i, :], ones_b[0:1, :], brow_lo[0:1, :], start=False, stop=True)
                return [s1, s2, s3, s4, s5]
            stages = [p5(i) for i in range(NT)]
            delays = [0, 0, 1, 1, 2]
            for t in range(NT + max(delays)):
                for j, dly in enumerate(delays):
                    k = t - dly
                    if 0 <= k < NT:
                        stages[k][j]()
            aff = SA.f32(NT * 16).rearrange("p (i e) -> p i e", e=16)
            bia = SA.f32(NT * 16).rearrange("p (i e) -> p i e", e=16)
            t16 = SA.f32(NT * 16).rearrange("p (i e) -> p i e", e=16)
            m1 = SA.f32(NT * 4)
            m2 = SA.f32(NT * 4)
            gs = SA.f32(NT * 4)
            gmx = SA.f32(NT)
            ing = SA.f32(NT * 4)
            ssm = SA.f32(NT)
            act(aff, plog, AF.Sigmoid)
            tt("dve", bia, aff, brt_bc.unsqueeze(1).to_broadcast([128, NT, 16]), ALU.add)
            bia4 = bia.rearrange("p i (g k) -> p (i g) k", g=4)
            t4 = t16.rearrange("p i (g k) -> p (i g) k", g=4)
            red(m1, bia4, ALU.max)
            tt("dve", t4, bia4, m1.unsqueeze(2).to_broadcast([128, NT * 4, 4]), ALU.is_equal)
            stt(t4, t4, -1.0e9, bia4, ALU.mult, ALU.add)
            red(m2, t4, ALU.max)
            tt("dve", gs, m1, m2, ALU.add)
            red(gmx, gs.rearrange("p (i g) -> p i g", g=4), ALU.max)
            tt("dve", ing.rearrange("p (i g) -> p i g", g=4), gs.rearrange("p (i g) -> p i g", g=4), gmx.unsqueeze(2).to_broadcast([128, NT, 4]), ALU.is_ge)
            tt("dve", t4, bia4, m2.unsqueeze(2).to_broadcast([128, NT * 4, 4]), ALU.is_ge)
            tt("dve", t4, t4, ing.unsqueeze(2).to_broadcast([128, NT * 4, 4]), ALU.mult)
            tt("dve", t16, t16, aff, ALU.mult)
            red(ssm, t16, ALU.add)
            recip(ssm, ssm)
            tt("dve", gates, t16, ssm.unsqueeze(2).to_broadcast([128, NT, 16]), ALU.mult)

        def phase6(l, s, SA, SB_, xseq, gates, g2bc):
            wg32 = [SA.f32(KC * DEXP).rearrange("p (k f) -> p k f", k=KC) for _ in range(2)]
            wu32 = [SA.f32(KC * DEXP).rearrange("p (k f) -> p k f", k=KC) for _ in range(2)]
            wd321 = SA.f32(2 * D).rearrange("p (c n) -> p c n", c=2)
            wd32 = [wd321, wd321]
            wgu = [SB_.bf(KC * 512).rearrange("p (k f) -> p k f", k=KC) for _ in range(2)]
            wdb = [SB_.bf(2 * D).rearrange("p (c n) -> p c n", c=2) for _ in range(2)]
            sgt = [SA.f32(512) for _ in range(2)]
            aT = [SB_.bf(2 * 512).rearrange("p (c t) -> p c t", c=2) for _ in range(2)]
            st6 = {"ndn": 0}
            items = []
            for e in range(NEXP):
                for tg in range(4):
                    def mk(e, tg):
                        p_ = e % 2
                        tsl = slice(tg * 512, (tg + 1) * 512)
                        a_ = aT[tg % 2]

                        def fa():
                            if tg == 0:
                                dma("sp", wg32[p_], wg_d[l, e].rearrange("(k p) f -> p k f", p=128))
                                dma("sp", wu32[p_], wu_d[l, e].rearrange("(k p) f -> p k f", p=128))
                                dma("sp", wd32[p_], wd_d[l, e].rearrange("(c p) n -> p c n", p=128))
                                cp("pool", wgu[p_][:, :, 0:256], wg32[p_])
                                cp("pool", wgu[p_][:, :, 256:512], wu32[p_])
                                tt("pool", wdb[p_], wd32[p_], g2bc.unsqueeze(1).to_broadcast([128, 2, D]), ALU.mult)
                            for half in range(2):
                                bg = half * 2
                                bu = half * 2 + 1
                                for kc in range(KC):
                                    mm(psb(bg), wgu[p_][:, kc, half * 128:(half + 1) * 128], hT[:, kc, tsl], start=(kc == 0), stop=(kc == KC - 1))
                                for kc in range(KC):
                                    mm(psb(bu), wgu[p_][:, kc, 256 + half * 128:256 + (half + 1) * 128], hT[:, kc, tsl], start=(kc == 0), stop=(kc == KC - 1))
                                act(sgt[half], psb(bg), AF.Silu)
                                tt("dve", a_[:, half, :], psb(bu), sgt[half], ALU.mult)

                        def fb():
                            for tl in range(4):
                                i = tg * 4 + tl
                                for nh in range(2):
                                    bank = 4 + (st6["ndn"] % 4)
                                    st6["ndn"] += 1
                                    for half in range(2):
                                        mm(psb(bank), a_[:, half, tl * 128:(tl + 1) * 128], wdb[p_][:, half, nh * 512:(nh + 1) * 512], start=(half == 0), stop=(half == 1))
                                    stt(xseq[:, i, nh * 512:(nh + 1) * 512], psb(bank), gates[:, i, e:e + 1], xseq[:, i, nh * 512:(nh + 1) * 512], ALU.mult, ALU.add)
                        return (fa, fb)
                    items.append(mk(e, tg))
            for f in pipelined(items, 1):
                f()

        def phase7(l, s, SA, xseq):
            if l < DEPTH - 1:
                dma("sp", xs_d[s].rearrange("(i p) d -> p i d", p=128), xseq)
                return
            gfb = SA.f32(D)
            dma("sp", gfb, gfin_d.partition_broadcast(128))
            junk = SA.bf(D)
            st2 = SA.f32(4)
            ot = [SA.f32(D) for _ in range(2)]
            for i in range(NT):
                ssum = st2[:, (i % 2) * 2:(i % 2) * 2 + 1]
                rstd = st2[:, (i % 2) * 2 + 1:(i % 2) * 2 + 2]
                rms_rstd(xseq[:, i, :], junk, ssum, rstd)
                stt(ot[i % 2], xseq[:, i, :], rstd, gfb, ALU.mult, ALU.mult)
                finals.append(dma("sp", out_d[s, i * 128:(i + 1) * 128, :], ot[i % 2]))

        def dump_bf(name, ap2d, n):
            if name in dbg_d:
                o = AW - n
                tmpf = sb[:, o:o + n]
                cp("dve", tmpf, ap2d)
                finals.append(dma("sp", dbg_d[name], tmpf))

        done = False
        if dbg:
            memset("pool", yT.rearrange("p k t -> p (k t)"), 0.0)
        if stop_after == "pro":
            layers = 0
        for l in range(layers):
            for s in range(2):
                first = (l == 0 and s == 0)
                load_vectors(l, s)
                if stop_after == "lv":
                    finals.append(dma("sp", dbg_d["hT"][:, 0:128], vecT))
                    done = True
                    break
                phase1(l, s, Alloc(S0, AW))
                if first:
                    dump_bf("hT", hT.rearrange("p k t -> p (k t)"), KC * L)
                if stop_after == "p1":
                    done = True
                    break
                phase2(l, s, Alloc(S0, AW))
                if stop_after == "p2":
                    if first:
                        dump_bf("yT", yT.rearrange("p k t -> p (k t)"), KC * L)
                    done = True
                    break
                phase3(l, s, Alloc(S0, AW))
                if stop_after == "p3":
                    if first:
                        dump_bf("yT", yT.rearrange("p k t -> p (k t)"), KC * L)
                    done = True
                    break
                phase4(l, s, Alloc(S0, AW))
                if first and stop_after == "p4":
                    dump_bf("yT", yT.rearrange("p k t -> p (k t)"), KC * L)
                    done = True
                    break
                SA = Alloc(S0, AW)
                xseq = SA.f32(NT * D).rearrange("p (i d) -> p i d", i=NT)
                gates = SA.f32(NT * 16).rearrange("p (i e) -> p i e", e=16)
                gbc_ = [SA.f32(D), SA.f32(D)]
                dma("sp", gbc_[0], mod_d[l, s, 2 * D:3 * D].partition_broadcast(128))
                dma("sp", gbc_[1], mod_d[l, s, 5 * D:6 * D].partition_broadcast(128))
                m5 = SA.o
                phase5(l, s, SA, xseq, gates, gbc_[0])
                if first and "xmid" in dbg_d:
                    finals.append(dma("sp", dbg_d["xmid"].rearrange("(i p) d -> p i d", p=128), xseq))
                if first and "gates" in dbg_d:
                    finals.append(dma("sp", dbg_d["gates"], gates.rearrange("p i e -> p (i e)")))
                if first and "h2T" in dbg_d and stop_after == "p5":
                    dump_bf("h2T", hT.rearrange("p k t -> p (k t)"), KC * L)
                if stop_after == "p5":
                    done = True
                    break
                yo = region(yT)[1] // 4
                phase6(l, s, Alloc(m5, AW), Alloc(yo, yo + KC * L // 2), xseq, gates, gbc_[1])
                if first and "x1" in dbg_d:
                    finals.append(dma("sp", dbg_d["x1"].rearrange("(i p) d -> p i d", p=128), xseq))
                if stop_after == "p6":
                    done = True
                    break
                phase7(l, s, Alloc(m5, AW), xseq)
            if done:
                break
        print("S0 words", S0, "ops", len(P.ops))
        P.emit(nc, final_wait_ops=finals)
    return nc


_CACHE = {}


def kernel(**inputs):
    ctab = make_ctab()
    kind = make_kind()
    if "nc" not in _CACHE:
        _CACHE["nc"] = build_program()
    nc = _CACHE["nc"]
    in_maps = []
    shared = {k: np.ascontiguousarray(np.asarray(v, dtype=np.float32)) for k, v in inputs.items() if k not in ("x", "c")}
    x = np.asarray(inputs["x"], dtype=np.float32)
    c = np.asarray(inputs["c"], dtype=np.float32)
    for core in range(NCORES):
        m = dict(shared)
        m["x"] = np.ascontiguousarray(x[2 * core:2 * core + 2])
        m["c"] = np.ascontiguousarray(c[2 * core:2 * core + 2])
        m["ctab"] = ctab
        m["kind"] = kind
        in_maps.append(m)
    res = run_bass_kernel_spmd(nc, in_maps, core_ids=list(range(NCORES)))
    out = np.concatenate([np.asarray(r["out"]) for r in res.results], axis=0)
    return out.astype(np.float32)
```

```python
import contextlib
import math
import numpy as np
import ml_dtypes
import concourse.bass as bass
import concourse.mybir as mybir
from concourse.bass_utils import run_bass_kernel_spmd

F32 = mybir.dt.float32
BF16 = mybir.dt.bfloat16
ALU = mybir.AluOpType
AF = mybir.ActivationFunctionType
AX = mybir.AxisListType

NCORES = 8
L = 2048
D = 1024
NT = L // 128
KC = D // 128
DEPTH = 2
IN_W = 2340
NEXP = 16
DEXP = 256
NEG = -30000.0
TOPK = 256
NBIS = 10

COMPUTE = ("pe", "act", "dve", "pool")


class Op:
    __slots__ = ("eng", "fn", "deps", "idx", "signal", "dma", "sem", "semval")

    def __init__(self, eng, fn, dma):
        self.eng = eng
        self.fn = fn
        self.deps = set()
        self.signal = False
        self.dma = dma
        self.sem = None
        self.semval = 0


def region(ap):
    t = ap.tensor
    dims = ap.ap
    off = ap.offset
    kind = type(t).__name__
    if kind.startswith("DRam"):
        ext = 1
        for (st, cnt) in dims:
            ext += (cnt - 1) * abs(st)
        return (t.name, off, off + ext, 0, 1)
    row, npart = dims[0]
    esz = mybir.dt.size(ap.dtype)
    if row == 0:
        row = 1 << 40
    p0 = off // row
    col0 = off % row
    ext = 1
    for (st, cnt) in dims[1:]:
        ext += (cnt - 1) * abs(st)
    lo = col0 * esz
    hi = (col0 + ext) * esz
    if kind.startswith("PSum"):
        return (t.name, (lo // 2048) * 2048, ((hi + 2047) // 2048) * 2048, 0, 128)
    return (t.name, lo, hi, p0, p0 + npart)


class Prog:
    def __init__(self):
        self.ops = []
        self.bufs = {}

    def add(self, eng, fn, reads=(), writes=(), dma=False):
        op = Op(eng, fn, dma)
        op.idx = len(self.ops)
        ops = self.ops
        ops.append(op)
        for ap in reads:
            name, lo, hi, p0, p1 = region(ap)
            b = self.bufs.setdefault(name, [[], []])
            for (a, c, q0, q1, w) in b[0]:
                if a < hi and lo < c and q0 < p1 and p0 < q1:
                    op.deps.add(w)
            if name == "ps":
                for (a, c, q0, q1, r) in b[1]:
                    if a < hi and lo < c and ops[r].eng != eng:
                        op.deps.add(r)
            if not dma:
                b[1] = [r for r in b[1] if not (ops[r[4]].eng == eng and not ops[r[4]].dma
                                                and lo <= r[0] and r[1] <= hi and p0 <= r[2] and r[3] <= p1)]
            b[1].append((lo, hi, p0, p1, op.idx))
        for ap in writes:
            name, lo, hi, p0, p1 = region(ap)
            b = self.bufs.setdefault(name, [[], []])
            for (a, c, q0, q1, w) in b[0]:
                if a < hi and lo < c and q0 < p1 and p0 < q1:
                    op.deps.add(w)
            for (a, c, q0, q1, r) in b[1]:
                if a < hi and lo < c and q0 < p1 and p0 < q1:
                    op.deps.add(r)
            b[0] = [w for w in b[0] if not (lo <= w[0] and w[1] <= hi and p0 <= w[2] and w[3] <= p1)]
            b[1] = [r for r in b[1] if not (lo <= r[0] and r[1] <= hi and p0 <= r[2] and r[3] <= p1)]
            b[0].append((lo, hi, p0, p1, op.idx))
        op.deps.discard(op.idx)
        return op

    def emit(self, nc, final_wait_ops=()):
        ops = self.ops
        for op in ops:
            for d in op.deps:
                dop = ops[d]
                if dop.eng == "pe" and op.eng == "pe" and not dop.dma and not op.dma:
                    continue
                dop.signal = True
        for op in final_wait_ops:
            op.signal = True
        for op in ops:
            if op.dma:
                op.signal = True
        engs = {"pe": nc.tensor, "act": nc.scalar, "dve": nc.vector, "pool": nc.gpsimd, "sp": nc.sync}
        with contextlib.ExitStack() as st:
            csem = {e: st.enter_context(nc.semaphore("s_" + e)) for e in COMPUTE}
            ccount = {e: 0 for e in COMPUTE}
            NDMA = 32
            NPOOL = 8
            dsem = [st.enter_context(nc.semaphore("d%d" % i)) for i in range(NDMA)]
            dcount = [0] * NDMA
            dlast = [None] * NDMA
            rr = {"pool": 0, "hw": 0}
            for op in ops:
                if op.dma:
                    if op.signal:
                        if op.eng == "pool":
                            k = rr["pool"] % NPOOL
                            rr["pool"] += 1
                        else:
                            k = NPOOL + rr["hw"] % (NDMA - NPOOL)
                            rr["hw"] += 1
                        if dlast[k] is not None:
                            op.deps.add(dlast[k].idx)
                        dcount[k] += 16
                        op.sem = dsem[k]
                        op.semval = dcount[k]
                        dlast[k] = op
                elif op.signal:
                    ccount[op.eng] += 1
                    op.sem = csem[op.eng]
                    op.semval = ccount[op.eng]
            per_eng = {e: [] for e in engs}
            for op in ops:
                per_eng[op.eng].append(op)
            block = st.enter_context(nc.Block())

            def run_stream(ename, eobj):
                seen = {}
                for op in per_eng[ename]:
                    need = {}
                    for d in op.deps:
                        dop = ops[d]
                        if dop.sem is None:
                            continue
                        if dop.eng == "pe" and ename == "pe" and not dop.dma and not op.dma:
                            continue
                        key = id(dop.sem)
                        if key not in need or need[key][1] < dop.semval:
                            need[key] = (dop.sem, dop.semval)
                    for key, (sem, val) in need.items():
                        if seen.get(key, 0) >= val:
                            continue
                        eobj.wait_ge(sem, val)
                        seen[key] = val
                    ins = op.fn(eobj)
                    if op.sem is not None:
                        ins.then_inc(op.sem, 16 if op.dma else 1)
                if ename == "sp":
                    for k in range(NDMA):
                        if dcount[k] > 0:
                            eobj.wait_ge(dsem[k], dcount[k])

            @block.tensor
            def _(e):
                run_stream("pe", e)

            @block.scalar
            def _(e):
                run_stream("act", e)

            @block.vector
            def _(e):
                run_stream("dve", e)

            @block.gpsimd
            def _(e):
                run_stream("pool", e)

            @block.sync
            def _(e):
                run_stream("sp", e)


CT_ID = 0
CT_TRINEG = 128
CT_TRIT = 256
CT_NEGI = 384
CT_CS64 = 512
CT_CS32 = 768
CT_PADC = 896
CT_NEGM = 1024
CT_POW2 = 1152
CT_OH = 1184
CT_W = 1184 + 2048


def make_ctab():
    t = np.zeros((128, CT_W), np.float32)
    p = np.arange(128)
    t[:, CT_ID:CT_ID + 128] = np.eye(128)
    t[:, CT_TRINEG:CT_TRINEG + 128] = np.where(p[None, :] <= p[:, None], 0.0, -3.0e38)
    t[:, CT_TRIT:CT_TRIT + 128] = np.where(p[:, None] <= p[None, :], 0.0, NEG)
    t[:, CT_NEGI:CT_NEGI + 128] = NEG * np.eye(128)
    pos = (np.arange(NT)[None, :] * 128 + p[:, None]).astype(np.float32)
    for (base, rot) in ((CT_CS64, 16), (CT_CS32, 8)):
        half = rot // 2
        inv = np.exp(np.float32(-math.log(500000.0)) * (np.arange(half, dtype=np.float32) * np.float32(2.0) / np.float32(rot))).astype(np.float32)
        ang = (pos[:, :, None] * inv[None, None, :]).astype(np.float32)
        cs = np.concatenate([np.cos(ang), np.sin(ang)], axis=-1).astype(np.float32)
        t[:, base:base + NT * rot] = cs.reshape(128, NT * rot)
    own = np.arange(NT) // 2
    n = np.arange(8)
    t[:, CT_PADC:CT_PADC + 128] = np.where(n[None, :] < own[:, None], 0.0, -1.0e30).reshape(1, 128)
    t[:, CT_NEGM:CT_NEGM + 128] = np.where(n[None, :] < own[:, None], NEG, 0.0).reshape(1, 128)
    t[:, CT_POW2:CT_POW2 + 32] = (2.0 ** -np.arange(32))[None, :]
    oh = np.zeros((128, 16, 128), np.float32)
    for e in range(16):
        oh[e, e, :] = 1.0
    t[:, CT_OH:CT_OH + 2048] = oh.reshape(128, 2048)
    return t


def make_kind():
    k = np.zeros((8, L), np.float32)
    for n in range(8):
        k[n, n * 256:(n + 1) * 256] = 1.0
    return k


def build_program(dbg=None, layers=DEPTH, stop_after=None):
    dbg = dbg or {}
    nc = bass.Bass("TRN2", target_bir_lowering=False)

    def din(name, shape):
        return nc.dram_tensor(name, list(shape), F32, kind="ExternalInput").ap()

    x_d = din("x", [2, L, D])
    c_d = din("c", [2, D])
    wada_d = din("w_ada", [DEPTH, D, 6 * D])
    bada_d = din("b_ada", [DEPTH, 6 * D])
    gmix_d = din("g_mix", [DEPTH, D])
    win_d = din("w_in", [DEPTH, D, IN_W])
    wdw_d = din("w_dw", [DEPTH, 31, 256])
    bdw_d = din("b_dw", [DEPTH, 256])
    lng_d = din("ln_conv_g", [DEPTH, 256])
    lnb_d = din("ln_conv_b", [DEPTH, 256])
    wo_d = din("w_o", [DEPTH, D, D])
    gffn_d = din("g_ffn", [DEPTH, D])
    wr_d = din("w_router", [D, NEXP])
    br_d = din("b_router", [NEXP])
    wg_d = din("w_gate", [DEPTH, NEXP, D, DEXP])
    wu_d = din("w_up", [DEPTH, NEXP, D, DEXP])
    wd_d = din("w_down", [DEPTH, NEXP, DEXP, D])
    gfin_d = din("g_final", [D])
    ctab_d = din("ctab", [128, CT_W])
    kind_d = din("kind", [8, L])
    out_d = nc.dram_tensor("out", [2, L, D], F32, kind="ExternalOutput").ap()
    xs_d = nc.dram_tensor("xs_scr", [2, L, D], F32).ap()
    mod_d = nc.dram_tensor("mod_scr", [DEPTH, 2, 6 * D], F32).ap()
    dbg_d = {k: nc.dram_tensor("dbg_" + k, list(shp), F32, kind="ExternalOutput").ap() for k, shp in dbg.items()}

    P = Prog()
    finals = []

    with contextlib.ExitStack() as st:
        AW = 53000
        sb = st.enter_context(nc.sbuf_tensor("arena", [128, AW], F32))
        ps = st.enter_context(nc.psum_tensor("ps", [128, 8, 512], F32))

        class Alloc:
            def __init__(self, base, limit):
                self.o = base
                self.limit = limit

            def f32(self, n):
                o = self.o
                self.o += n
                assert self.o <= self.limit, (self.o, self.limit)
                return sb[:, o:o + n]

            def bf(self, n):
                w = (n + 1) // 2
                o = self.o
                self.o += w
                assert self.o <= self.limit, (self.o, self.limit)
                return sb[:, o:o + w].bitcast(BF16)

        def aps(*xs):
            return [x for x in xs if x is not None and not isinstance(x, (int, float))]

        def mm(out, lhsT, rhs, start=True, stop=True):
            P.add("pe", lambda e: e.matmul(out, lhsT=lhsT, rhs=rhs, start=start, stop=stop), reads=[lhsT, rhs], writes=[out])

        def tr(out, in_, ident):
            P.add("pe", lambda e: e.transpose(out=out, in_=in_, identity=ident), reads=[in_, ident], writes=[out])

        def act(out, in_, func, scale=1.0, bias=None, accum=None):
            kw = {}
            if bias is not None:
                kw["bias"] = bias
            if accum is not None:
                kw["accum_out"] = accum
            P.add("act", lambda e: e.activation(out=out, in_=in_, func=func, scale=scale, **kw),
                  reads=aps(in_, scale, bias), writes=aps(out, accum))

        def ts(eng, out, in0, s1, s2=None, op0=ALU.mult, op1=None, accum=None):
            kw = {}
            if op1 is not None:
                kw["op1"] = op1
            if accum is not None:
                kw["accum_out"] = accum
            P.add(eng, lambda e: e.tensor_scalar(out=out, in0=in0, scalar1=s1, scalar2=s2, op0=op0, **kw),
                  reads=aps(in0, s1, s2), writes=aps(out, accum))

        def tt(eng, out, in0, in1, op):
            P.add(eng, lambda e: e.tensor_tensor(out=out, in0=in0, in1=in1, op=op), reads=[in0, in1], writes=[out])

        def stt(out, in0, scalar, in1, op0, op1):
            P.add("dve", lambda e: e.scalar_tensor_tensor(out=out, in0=in0, scalar=scalar, in1=in1, op0=op0, op1=op1),
                  reads=aps(in0, scalar, in1), writes=[out])

        def cp(eng, out, in_):
            if eng == "act":
                P.add("act", lambda e: e.activation(out=out, in_=in_, func=AF.Copy), reads=[in_], writes=[out])
            else:
                P.add(eng, lambda e: e.tensor_copy(out=out, in_=in_), reads=[in_], writes=[out])

        def memset(eng, out, val):
            P.add(eng, lambda e: e.memset(out, val), writes=[out])

        def dma(eng, out, in_):
            return P.add(eng, lambda e: e.dma_start(out=out, in_=in_), reads=[in_], writes=[out], dma=True)

        def red(out, in_, op, axis=AX.X):
            P.add("dve", lambda e: e.tensor_reduce(out=out, in_=in_, axis=axis, op=op), reads=[in_], writes=[out])

        def recip(out, in_):
            P.add("dve", lambda e: e.reciprocal(out=out, in_=in_), reads=[in_], writes=[out])

        def max8(out, in_):
            P.add("dve", lambda e: e.max(out=out, in_=in_), reads=[in_], writes=[out])

        def dump(name, ap_sb, dram_ap=None):
            if name in dbg_d:
                finals.append(dma("sp", dram_ap if dram_ap is not None else dbg_d[name], ap_sb))

        def psb(b, lo=0, hi=512):
            return ps[:, b, lo:hi]

        def psbf(b):
            return ps[:, b, :].bitcast(BF16)

        A = Alloc(0, AW)
        ctab = A.f32(CT_OH)
        ident_f = ctab[:, CT_ID:CT_ID + 128]
        trineg = ctab[:, CT_TRINEG:CT_TRINEG + 128]
        cs64 = ctab[:, CT_CS64:CT_CS64 + 256].rearrange("p (i r) -> p i r", r=16)
        cs32 = ctab[:, CT_CS32:CT_CS32 + 128].rearrange("p (i r) -> p i r", r=8)
        padc = ctab[:, CT_PADC:CT_PADC + 128].rearrange("p (i n) -> p i n", n=8)
        negm = ctab[:, CT_NEGM:CT_NEGM + 128].rearrange("p (i n) -> p i n", n=8)
        pow2 = ctab[:, CT_POW2:CT_POW2 + 32]
        ident_b = A.bf(128)
        trit_b = A.bf(128)
        negi3 = A.bf(384).rearrange("p (g q) -> p g q", g=3)
        onesq_b = A.bf(128)
        ones_b = A.bf(128)
        oh_b = A.bf(2048).rearrange("p (e m) -> p e m", e=16)
        cst = A.f32(8)
        rows = A.f32(128)
        vecT = A.f32(128)
        gm = A.f32(16)
        brt_bc = A.f32(16)
        wrT = A.f32(128)
        wr_hi = A.bf(128)
        wr_lo = A.bf(128)
        brow = A.f32(16)
        brow_hi = A.bf(16)
        brow_lo = A.bf(16)
        cactT = A.bf(16)
        sh2col = A.bf(8)
        hT = A.bf(KC * L).rearrange("p (k t) -> p k t", k=KC)
        yT = A.bf(KC * L).rearrange("p (k t) -> p k t", k=KC)
        S0 = A.o

        dma("sp", ctab, ctab_d[:, 0:CT_OH])
        cp("pool", ident_b, ident_f)
        cp("pool", trit_b, ctab[:, CT_TRIT:CT_TRIT + 128])
        for g in range(3):
            cp("pool", negi3[:, g, :], ctab[:, CT_NEGI:CT_NEGI + 128])
        memset("pool", onesq_b, 1.0 / 256.0)
        memset("pool", ones_b, 1.0)
        ohf = sb[:, S0:S0 + 2048]
        dma("sp", ohf, ctab_d[:, CT_OH:CT_OH + 2048])
        cp("pool", oh_b.rearrange("p e m -> p (e m)"), ohf)
        memset("pool", cst[:, 0:1], 1e-6)
        memset("pool", rows, 0.0)
        dma("sp", brt_bc, br_d.partition_broadcast(128))
        for kc in range(KC):
            dma("sp", wrT[:, kc * 16:(kc + 1) * 16], wr_d[kc * 128:(kc + 1) * 128, :])

        PA = Alloc(S0 + 2048, AW)
        cin = PA.f32(128)
        dma("sp", cin[0:16, :], c_d.rearrange("b (k p) -> (b k) p", p=128))
        act(cin[0:16, :], cin[0:16, :], AF.Silu)
        tr(psb(0, 0, 16), cin[0:16, :], ident_f[0:16, 0:16])
        cp("dve", cactT, psb(0, 0, 16))
        cactT3 = cactT.rearrange("p (b k) -> p b k", b=2)
        cbc = PA.bf(KC * 128).rearrange("p (k m) -> p k m", k=KC)
        for b in range(2):
            cp("dve", cbc[:, :, b * 64:(b + 1) * 64], cactT3[:, b, :].unsqueeze(2).to_broadcast([128, KC, 64]))
        wada_b = PA.bf(KC * 1536).rearrange("p (k n) -> p k n", k=KC)
        bada = PA.f32(6 * D)
        modrow = PA.f32(6 * D)
        import os
        PRO = os.environ.get("PRO", "all")
        for l in range(DEPTH if PRO != "nowada" else 0):
            dma("sp", bada, bada_d[l].partition_broadcast(128))
            for q in range(4):
                for kc in range(KC):
                    dma("pool", wada_b[:, kc, :], wada_d[l, kc * 128:(kc + 1) * 128, q * 1536:(q + 1) * 1536])
                for j in range(3):
                    bank = (q * 3 + j) % 2
                    c0 = q * 1536 + j * 512
                    for kc in range(KC):
                        mm(psb(bank), cbc[:, kc, :], wada_b[:, kc, j * 512:(j + 1) * 512], start=(kc == 0), stop=(kc == KC - 1))
                    tt("dve", modrow[:, c0:c0 + 512], psb(bank), bada[:, c0:c0 + 512], ALU.add)
            dma("sp", mod_d[l, 0:1, :], modrow[0:1, :])
            dma("sp", mod_d[l, 1:2, :], modrow[64:65, :])
            if l == 0 and "mod0" in dbg_d:
                finals.append(dma("sp", dbg_d["mod0"][0:1, :], modrow[0:1, :]))
                finals.append(dma("sp", dbg_d["mod0"][1:2, :], modrow[64:65, :]))

        def load_vectors(l, s):
            srcs = [mod_d[l, s, 0:D], mod_d[l, s, D:2 * D], mod_d[l, s, 3 * D:4 * D], mod_d[l, s, 4 * D:5 * D],
                    gmix_d[l], gffn_d[l]]
            for j, src in enumerate(srcs):
                dma("sp", rows[8 * j:8 * j + 8, :], src.rearrange("(c p) -> c p", p=128))
            dma("sp", rows[48:79, :], wdw_d[l, :, 0:128])
            dma("sp", rows[79:110, :], wdw_d[l, :, 128:256])
            dma("sp", rows[110:112, :], bdw_d[l].rearrange("(c p) -> c p", p=128))
            dma("sp", rows[112:114, :], lng_d[l].rearrange("(c p) -> c p", p=128))
            dma("sp", rows[114:116, :], lnb_d[l].rearrange("(c p) -> c p", p=128))
            tr(psb(7, 0, 128), rows, ident_f)
            cp("dve", vecT, psb(7, 0, 128))
            stt(gm[:, 0:8], vecT[:, 8:16], 1.0, vecT[:, 32:40], ALU.add, ALU.mult)
            stt(gm[:, 8:16], vecT[:, 24:32], 1.0, vecT[:, 40:48], ALU.add, ALU.mult)

        sh1 = vecT[:, 0:8]
        sh2 = vecT[:, 16:24]
        wdwT = [vecT[:, 48:79], vecT[:, 79:110]]
        bdwT = vecT[:, 110:112]
        lngT = vecT[:, 112:114]
        lnbT = vecT[:, 114:116]

        def rms_rstd(xt, junk, ssum, rstd):
            act(junk, xt, AF.Square, accum=ssum)
            act(ssum, ssum, AF.Sqrt, scale=1.0 / D, bias=cst[:, 0:1])
            recip(rstd, ssum)

        def phase1(l, s, SA, emit=True):
            xsrc = x_d if l == 0 else xs_d
            xt = [SA.f32(D) for _ in range(2)]
            xn = [SA.bf(D) for _ in range(2)]
            junk = SA.bf(D)
            st2 = SA.f32(4)
            def p1(i):
                x_t = xt[i % 2]
                ssum = st2[:, (i % 2) * 2:(i % 2) * 2 + 1]
                rstd = st2[:, (i % 2) * 2 + 1:(i % 2) * 2 + 2]

                def fa():
                    dma("sp", x_t, xsrc[s, i * 128:(i + 1) * 128, :])
                    rms_rstd(x_t, junk, ssum, rstd)
                    act(xn[i % 2], x_t, AF.Identity, scale=rstd)

                def fb():
                    pb = psbf(i % 2).rearrange("p (k t) -> p k t", k=KC)
                    for kc in range(KC):
                        tr(pb[:, kc, :], xn[i % 2][:, kc * 128:(kc + 1) * 128], ident_b)
                    for kc in range(KC):
                        if i % 2 == 0:
                            act(hT[:, kc, i * 128:(i + 1) * 128], pb[:, kc, :], AF.Identity, scale=gm[:, kc:kc + 1], bias=sh1[:, kc:kc + 1])
                        else:
                            ts("dve", hT[:, kc, i * 128:(i + 1) * 128], pb[:, kc, :], gm[:, kc:kc + 1], sh1[:, kc:kc + 1], ALU.mult, ALU.add)
                return (fa, fb)
            cl = pipelined([p1(i) for i in range(NT)], 1)
            if not emit:
                return cl
            for f in cl:
                f()

        def load_win(l, c0, c1, SA):
            w = SA.bf(KC * (c1 - c0)).rearrange("p (k n) -> p k n", k=KC)
            for kc in range(KC):
                dma("pool", w[:, kc, :], win_d[l, kc * 128:(kc + 1) * 128, c0:c1])
            return w

        def phase2(l, s, SA, p1cl=None):
            w = load_win(l, 0, 512, SA)
            apad = SA.bf(2 * (L + 32)).rearrange("p (c t) -> p c t", c=2)
            dg = SA.bf(2 * 31 * 128).rearrange("p (c j m) -> p c j m", c=2, j=31)
            acc = SA.f32(2 * L).rearrange("p (c t) -> p c t", c=2)
            sg = [SA.f32(512) for _ in range(2)]
            ybf = SA.bf(2 * 512).rearrange("p (c t) -> p c t", c=2)
            ysq = SA.bf(2 * 512).rearrange("p (c t) -> p c t", c=2)
            tmp = SA.f32(512)
            rstd = SA.f32(512)
            dd = [SA.f32(512) for _ in range(2)]
            memset("pool", apad[:, :, 0:32], 0.0)
            for cc in range(2):
                for j in range(31):
                    ts("dve" if j % 2 == 0 else "pool", dg[:, cc, j, :], ident_f, wdwT[cc][:, j:j + 1], 1.0, ALU.mult, ALU.mult)

            def glu(tg):
                tsl = slice(tg * 512, (tg + 1) * 512)
                for cc in range(2):
                    for half, col in ((0, cc * 128), (1, 256 + cc * 128)):
                        for kc in range(KC):
                            mm(psb(2 + half), w[:, kc, col:col + 128], hT[:, kc, tsl], start=(kc == 0), stop=(kc == KC - 1))
                    act(sg[cc], psb(3), AF.Sigmoid)
                    tt("dve", apad[:, cc, 32 + tg * 512:32 + (tg + 1) * 512], psb(2), sg[cc], ALU.mult)

            def conv(tg):
                tsl = slice(tg * 512, (tg + 1) * 512)
                for cc in range(2):
                    for j in range(31):
                        o = 2 + j + tg * 512
                        mm(psb(4 + cc), dg[:, cc, j, :], apad[:, cc, o:o + 512], start=(j == 0), stop=(j == 30))
                    act(acc[:, cc, tsl], psb(4 + cc), AF.Identity, bias=bdwT[:, cc:cc + 1])
                    cp("dve", ybf[:, cc, :], acc[:, cc, tsl])
                    act(ysq[:, cc, :], acc[:, cc, tsl], AF.Square)
                for cc in range(2):
                    mm(psb(6), onesq_b, ybf[:, cc, :], start=(cc == 0), stop=(cc == 1))
                for cc in range(2):
                    mm(psb(7), onesq_b, ysq[:, cc, :], start=(cc == 0), stop=(cc == 1))
                cp("act", tmp, psb(6))
                tt("dve", rstd, tmp, tmp, ALU.mult)
                tt("dve", rstd, psb(7), rstd, ALU.subtract)
                ts("dve", rstd, rstd, 0.0, None, ALU.max)
                act(rstd, rstd, AF.Sqrt, bias=cst[:, 0:1])
                recip(rstd, rstd)
                for cc in range(2):
                    tt("dve", dd[cc], acc[:, cc, tsl], tmp, ALU.subtract)
                    tt("dve", dd[cc], dd[cc], rstd, ALU.mult)
                    act(yT[:, cc, tsl], dd[cc], AF.Silu, scale=lngT[:, cc:cc + 1], bias=lnbT[:, cc:cc + 1])

            if p1cl is None:
                for tg in range(4):
                    glu(tg)
                for tg in range(4):
                    conv(tg)
                return
            for t, f in enumerate(p1cl):
                f()
                if t >= 4 and t % 4 == 0:
                    glu(t // 4 - 1)
                    conv(t // 4 - 1)

        def rope(dst, src, nh, hd, half, cs_i, tmp):
            cos = cs_i[:, 0:half].unsqueeze(1).to_broadcast([128, nh, half])
            sin = cs_i[:, half:2 * half].unsqueeze(1).to_broadcast([128, nh, half])
            x1 = src[:, :, 0:half]
            x2 = src[:, :, half:2 * half]
            t1 = tmp[:, 0:nh * half].rearrange("p (h r) -> p h r", h=nh)
            t2 = tmp[:, 64:64 + nh * half].rearrange("p (h r) -> p h r", h=nh)
            t3 = tmp[:, 128:128 + nh * half].rearrange("p (h r) -> p h r", h=nh)
            t4 = tmp[:, 192:192 + nh * half].rearrange("p (h r) -> p h r", h=nh)
            tt("dve", t1, x1, cos, ALU.mult)
            tt("dve", t2, x2, sin, ALU.mult)
            tt("dve", t3, x2, cos, ALU.mult)
            tt("dve", t4, x1, sin, ALU.mult)
            tt("dve", dst[:, :, 0:half], t1, t2, ALU.subtract)
            tt("dve", dst[:, :, half:2 * half], t3, t4, ALU.add)
            cp("act", dst[:, :, 2 * half:hd], src[:, :, 2 * half:hd])

        def pipelined(items, depth):
            out = []
            n = len(items)
            for k in range(n + depth):
                def f(k=k):
                    if k < n and items[k][0] is not None:
                        items[k][0]()
                    if k - depth >= 0 and items[k - depth][1] is not None:
                        items[k - depth][1]()
                out.append(f)
            return out

        def phase3(l, s, SA):
            qT = SA.bf(6 * L).rearrange("p (h t) -> p h t", h=6)
            kT = SA.bf(L)
            vd = SA.bf(NT * 128).rearrange("p (c m) -> p c m", c=NT)
            vds = SA.bf(NT * 128).rearrange("p (c m) -> p c m", c=NT)
            iqT = SA.bf(4 * L).rearrange("p (h t) -> p h t", h=4)
            ikT = SA.bf(L)
            widx = SA.f32(NT * 4).rearrange("p (i h) -> p i h", h=4)
            mark = SA.o
            w = load_win(l, 512, 1188, SA)
            qs = [SA.bf(7 * 64).rearrange("p (h d) -> p h d", h=7) for _ in range(2)]
            iqs = [SA.bf(5 * 32).rearrange("p (h d) -> p h d", h=5) for _ in range(2)]
            rtmp = [SA.f32(256) for _ in range(2)]
            rtmp2 = [SA.f32(256) for _ in range(2)]
            memset("pool", vd[:, :, 64:128], 1.0)
            memset("pool", vds[:, :, 0:64], 1.0)
            slot = {0: 0, 2: 1, 4: 2, 1: 3, 3: 4, 5: 5}
            def p3a(i):
                tsl = slice(i * 128, (i + 1) * 128)
                b0 = (i % 2) * 2

                def fa():
                    for (bank, c0, c1) in ((b0, 0, 512), (b0 + 1, 512, 676)):
                        for kc in range(KC):
                            mm(psb(bank, 0, c1 - c0), hT[:, kc, tsl], w[:, kc, c0:c1], start=(kc == 0), stop=(kc == KC - 1))
                    pq = psb(b0, 0, 448).rearrange("p (h d) -> p h d", h=7)
                    rope(qs[i % 2], pq, 7, 64, 8, cs64[:, i, :], rtmp[i % 2])
                    cp("act", vd[:, i, 0:64], psb(b0, 448, 512))
                    cp("dve", vds[:, i, 64:128], psb(b0, 448, 512))
                    piq = psb(b0 + 1, 0, 160).rearrange("p (h d) -> p h d", h=5)
                    rope(iqs[i % 2], piq, 5, 32, 4, cs32[:, i, :], rtmp2[i % 2])
                    cp("act", widx[:, i, :], psb(b0 + 1, 160, 164))

                def fb():
                    pt = psbf(4 + (i % 2))
                    ptq = pt[:, 0:768].rearrange("p (h t) -> p h t", h=6)
                    for h in range(6):
                        tr(ptq[0:64, slot[h], :], qs[i % 2][:, h, :], ident_b)
                    tr(pt[0:64, 768:896], qs[i % 2][:, 6, :], ident_b)
                    cp("act", qT[0:64, :, tsl], ptq[0:64, :, :])
                    cp("dve", kT[0:64, tsl], pt[0:64, 768:896])
                    pt2 = psbf(6 + (i % 2))
                    pti = pt2[:, 0:512].rearrange("p (h t) -> p h t", h=4)
                    for h in range(4):
                        tr(pti[0:32, h, :], iqs[i % 2][:, h, :], ident_b)
                    tr(pt2[0:32, 512:640], iqs[i % 2][:, 4, :], ident_b)
                    cp("act", iqT[0:32, :, tsl], pti[0:32, :, :])
                    cp("dve", ikT[0:32, tsl], pt2[0:32, 512:640])
                return (fa, fb)
            for f in pipelined([p3a(i) for i in range(NT)], 1):
                f()
            SA.o = mark
            score = [SA.f32(L) for _ in range(4)]
            rel = [SA.f32(512) for _ in range(2)]
            junk = [SA.bf(L) for _ in range(2)]
            notsel = [SA.bf(L) for _ in range(4)]
            PT = [SA.bf(768).rearrange("p (g q) -> p g q", g=2) for _ in range(3)]
            bst = [SA.f32(64) for _ in range(4)]
            rec = SA.f32(384)
            otS1 = SA.f32(768).rearrange("p (g q) -> p g q", g=2)
            otS = [otS1, otS1]
            cnt_ = {"npt": 0, "lb": 0}

            def score_chunks(i):
                S = 128 * (i + 1)
                sc = score[i % 4]
                tsl = slice(i * 128, (i + 1) * 128)
                nchunk = (S + 511) // 512
                out = []

                def mk(ch):
                    def f():
                        k0 = ch * 512
                        k1 = min(S, k0 + 512)
                        for h in range(4):
                            bank = cnt_["lb"] % 2
                            cnt_["lb"] += 1
                            mm(psb(bank, 0, k1 - k0), iqT[0:32, h, tsl], ikT[0:32, k0:k1])
                            if h == 0:
                                ts("dve", sc[:, k0:k1], psb(bank, 0, k1 - k0), 0.0, widx[:, i, 0:1], ALU.max, ALU.mult)
                            else:
                                r = rel[h % 2]
                                act(r[:, 0:k1 - k0], psb(bank, 0, k1 - k0), AF.Relu)
                                stt(sc[:, k0:k1], r[:, 0:k1 - k0], widx[:, i, h:h + 1], sc[:, k0:k1], ALU.mult, ALU.add)
                        if ch == nchunk - 1:
                            tt("dve", sc[:, i * 128:S], sc[:, i * 128:S], trineg, ALU.add)
                    return f
                for ch in range(nchunk):
                    out.append(mk(ch))
                return out

            def bisect_init(i):
                def f():
                    sc = score[i % 4]
                    bs = bst[i % 4]
                    pre = sc[:, 0:i * 128]
                    mx = bs[:, 0:1]
                    mn = bs[:, 1:2]
                    cand = bs[:, 2:3]
                    steps = bs[:, 8:8 + NBIS + 1]
                    red(mx, pre, ALU.max)
                    red(mn, pre, ALU.min)
                    tt("dve", mx, mx, mn, ALU.subtract)
                    ts("dve", steps, pow2[:, 1:NBIS + 2], mx, None, ALU.mult)
                    tt("dve", cand, mn, steps[:, 0:1], ALU.add)
                return f

            def bisect_steps(i, engs):
                S = 128 * (i + 1)
                sc = score[i % 4]
                bs = bst[i % 4]
                cand = bs[:, 2:3]
                cnt = bs[:, 3:4]
                dlt = bs[:, 4:5]
                steps = bs[:, 8:8 + NBIS + 1]
                out = []

                def mk(it):
                    def f():
                        if engs[it] == "dve":
                            ts("dve", junk[0][:, 0:S], sc[:, 0:S], cand, None, ALU.is_ge, ALU.add, accum=cnt)
                            ts("dve", dlt, cnt, TOPK - 0.5, 0.5, ALU.is_ge, ALU.subtract)
                        else:
                            act(junk[1][:, 0:S], sc[:, 0:S], AF.Sign, scale=-1.0, bias=cand, accum=cnt)
                            ts("dve", dlt, cnt, S - 2 * TOPK + 1.0, 0.5, ALU.is_le, ALU.subtract)
                        if it == NBIS - 1:
                            ts("dve", dlt, dlt, 0.5, None, ALU.subtract)
                        stt(cand, dlt, steps[:, it:it + 1], cand, ALU.mult, ALU.add)
                    return f
                for it in range(NBIS):
                    out.append(mk(it))
                return out

            def finish(i):
                S = 128 * (i + 1)
                sc = score[i % 4]
                ns = notsel[i % 4]
                if i >= 2:
                    ts("dve", ns[:, 0:S], sc[:, 0:S], bst[i % 4][:, 2:3], None, ALU.is_lt)
                else:
                    ts("dve", ns[:, 0:S], sc[:, 0:S], -1.0e37, None, ALU.is_lt)

            def attention(i):
                tsl = slice(i * 128, (i + 1) * 128)
                ns = notsel[i % 4]
                items = []

                def mk(c):
                    st_ = {}

                    def fa():
                        csl = slice(c * 128, (c + 1) * 128)
                        st_["sb0"] = 2 + 2 * (cnt_["npt"] % 2)
                        st_["pt"] = PT[cnt_["npt"] % 3]
                        cnt_["npt"] += 1
                        for g in range(2):
                            mm(psb(st_["sb0"] + g, 0, 384), kT[0:64, csl], qT[0:64, 3 * g:3 * g + 3, tsl], start=True, stop=False)
                            mm(psb(st_["sb0"] + g, 0, 384), ns[:, csl], negi3, start=False, stop=True)

                    def fb():
                        act(st_["pt"], ps[:, st_["sb0"]:st_["sb0"] + 2, 0:384], AF.Exp, scale=0.125)
                        for g in range(2):
                            mm(psb(6 + g, 0, 384), (vd if g == 0 else vds)[:, c, :], st_["pt"][:, g, :], start=(c == 0), stop=(c == i))
                    return (fa, fb)

                def norm():
                    o_ = otS[i % 2]
                    cp("dve", o_, ps[:, 6:8, 0:384])
                    act(rec[0:64, :], o_[64:128, 0, :], AF.Ln)
                    act(rec[0:64, :], rec[0:64, :], AF.Exp, scale=-1.0)
                    tt("dve", yT[0:64, 2:5, tsl], o_[0:64, 0, :].rearrange("p (h q) -> p h q", h=3), rec[0:64, :].rearrange("p (h q) -> p h q", h=3), ALU.mult)
                    act(rec[64:128, :], o_[0:64, 1, :], AF.Ln)
                    act(rec[64:128, :], rec[64:128, :], AF.Exp, scale=-1.0)
                    tt("dve", yT[64:128, 2:5, tsl], o_[64:128, 1, :].rearrange("p (h q) -> p h q", h=3), rec[64:128, :].rearrange("p (h q) -> p h q", h=3), ALU.mult)
                for c in range(i + 1):
                    items.append(mk(c))
                items.append((None, norm))
                return items

            for f in score_chunks(0) + score_chunks(1):
                f()
            pending = []
            for j in range(NT // 2):
                ta, tb = 2 * j, 2 * j + 1
                sa = bisect_steps(ta, ["act" if it % 2 == 1 else "dve" for it in range(NBIS)]) if ta >= 2 else []
                sb_ = bisect_steps(tb, ["act"] * NBIS) if tb >= 2 else []
                nxt = []
                if j + 1 < NT // 2:
                    nxt = score_chunks(ta + 2) + score_chunks(tb + 2) + [bisect_init(ta + 2), bisect_init(tb + 2)]
                nr = NBIS if sa else 1
                for it in range(nr):
                    if sb_:
                        sb_[it]()
                    if sa:
                        sa[it]()
                    left = nr - it
                    for _ in range((len(pending) + left - 1) // left):
                        pending.pop(0)()
                    for _ in range((len(nxt) + left - 1) // left):
                        nxt.pop(0)()
                while pending:
                    pending.pop(0)()
                while nxt:
                    nxt.pop(0)()
                finish(ta)
                finish(tb)
                pending = pipelined(attention(ta) + attention(tb), 1)
            while pending:
                pending.pop(0)()

        def phase4(l, s, SA):
            w = load_win(l, 1188, 2340, SA)
            qa = SA.bf(6 * L).rearrange("p (h t) -> p h t", h=6)
            ka = SA.bf(6 * L).rearrange("p (h t) -> p h t", h=6)
            vm = SA.bf(NT * 6 * 128).rearrange("p (c h m) -> p c h m", c=NT, h=6)
            kmT = SA.bf(6 * 8).rearrange("p (h n) -> p h n", h=6)
            kms = SA.f32(6 * 8).rearrange("p (h n) -> p h n", h=6)
            qs = [SA.bf(6 * 64).rearrange("p (h d) -> p h d", h=6) for _ in range(2)]
            ks = [SA.bf(6 * 64).rearrange("p (h d) -> p h d", h=6) for _ in range(2)]
            qb = [SA.bf(6 * 72).rearrange("p (h d) -> p h d", h=6) for _ in range(2)]
            rtmp = [SA.f32(256) for _ in range(2)]
            rtmp2 = [SA.f32(256) for _ in range(2)]
            gmk = [SA.f32(48).rearrange("p (h n) -> p h n", h=6) for _ in range(2)]
            g8 = [SA.f32(48).rearrange("p (h n) -> p h n", h=6) for _ in range(2)]
            kindf = SA.f32(L)
            dma("sp", kindf[64:72, :], kind_d)
            for h in range(6):
                cp("pool", ka[64:72, h, :], kindf[64:72, :])
            for par in range(2):
                memset("pool", qb[par], 0.0)
            for h in range(6):
                if h % 2 == 0:
                    memset("pool", vm[:, :, h, 64:128], 1.0)
                else:
                    memset("pool", vm[:, :, h, 0:64], 1.0)
            def p4a(i):
                tsl = slice(i * 128, (i + 1) * 128)
                b0 = (i % 2) * 3

                def fa():
                    for j in range(3):
                        for kc in range(KC):
                            mm(psb(b0 + j, 0, 384), hT[:, kc, tsl], w[:, kc, j * 384:(j + 1) * 384], start=(kc == 0), stop=(kc == KC - 1))
                    pq = psb(b0, 0, 384).rearrange("p (h d) -> p h d", h=6)
                    pk = psb(b0 + 1, 0, 384).rearrange("p (h d) -> p h d", h=6)
                    pv = psb(b0 + 2, 0, 384).rearrange("p (h d) -> p h d", h=6)
                    rope(qs[i % 2], pq, 6, 64, 8, cs64[:, i, :], rtmp[i % 2])
                    rope(ks[i % 2], pk, 6, 64, 8, cs64[:, i, :], rtmp2[i % 2])
                    for h in range(6):
                        o = 0 if h % 2 == 0 else 64
                        cp("act" if h % 2 == 0 else "dve", vm[:, i, h, o:o + 64], pv[:, h, :])

                def fb():
                    ptq = psbf(6)[:, 0:768].rearrange("p (h t) -> p h t", h=6)
                    ptk = psbf(7)[:, 0:768].rearrange("p (h t) -> p h t", h=6)
                    for h in range(6):
                        tr(ptq[0:64, h, :], qs[i % 2][:, h, :], ident_b)
                    for h in range(6):
                        tr(ptk[0:64, h, :], ks[i % 2][:, h, :], ident_b)
                    cp("act", qa[0:64, :, tsl], ptq[0:64, :, :])
                    cp("dve", ka[0:64, :, tsl], ptk[0:64, :, :])
                return (fa, fb)
            for f in pipelined([p4a(i) for i in range(NT)], 1):
                f()
            for h in range(6):
                red(kms[0:64, h, :], ka[0:64, h, :].rearrange("p (n k) -> p n k", n=8), ALU.add)
            ts("dve", kmT[0:64, :, :], kms[0:64, :, :], 1.0 / 256.0, None, ALU.mult)
            def p4g(i):
                tsl = slice(i * 128, (i + 1) * 128)

                def fa():
                    pg = psb(i % 2, 0, 48).rearrange("p (h n) -> p h n", h=6)
                    for h in range(6):
                        mm(pg[:, h, :], qa[0:64, h, tsl], kmT[0:64, h, :])
                    g_ = gmk[i % 2]
                    tt("dve", g_, pg, padc[:, i, :].unsqueeze(1).to_broadcast([128, 6, 8]), ALU.add)
                    for h in range(6):
                        max8(g8[i % 2][:, h, :], g_[:, h, :])
                    tt("dve", g_, g_, g8[i % 2][:, :, 2:3].to_broadcast([128, 6, 8]), ALU.is_lt)
                    tt("dve", qb[i % 2][:, :, 64:72], g_, negm[:, i, :].unsqueeze(1).to_broadcast([128, 6, 8]), ALU.mult)

                def fb():
                    pb = psbf(2 + (i % 2))[:, 0:768].rearrange("p (h t) -> p h t", h=6)
                    for h in range(6):
                        tr(pb[0:72, h, :], qb[i % 2][:, h, :], ident_b)
                    cp("act", qa[64:72, :, tsl], pb[64:72, :, :])
                return (fa, fb)
            for f in pipelined([p4g(i) for i in range(NT)], 1):
                f()
            PT = [SA.bf(512) for _ in range(4)]
            rec = [SA.f32(512) for _ in range(2)]
            st4 = {"npt": 0, "nacc": 0}
            items = []
            for h in range(6):
                odd = h % 2
                for qg in range(4):
                    nch = 4 * qg + 4
                    grp = {}

                    def mk(h, qg, c, nch, grp):
                        st_ = {}

                        def fa():
                            if c == 0:
                                grp["ob"] = 4 + (st4["nacc"] % 2)
                                st4["nacc"] += 1
                            csl = slice(c * 128, (c + 1) * 128)
                            col0 = 0 if c < 4 * qg else 128 * (c - 4 * qg)
                            st_["col0"] = col0
                            st_["sbk"] = st4["npt"] % 4
                            st_["pt"] = PT[st4["npt"] % 4]
                            st4["npt"] += 1
                            diag = c >= 4 * qg
                            mm(psb(st_["sbk"], col0, 512), ka[0:72, h, csl], qa[0:72, h, qg * 512 + col0:(qg + 1) * 512], start=True, stop=not diag)
                            if diag:
                                mm(psb(st_["sbk"], col0, col0 + 128), ident_b, trit_b, start=False, stop=True)

                        def fb():
                            col0 = st_["col0"]
                            act(st_["pt"][:, col0:512], psb(st_["sbk"], col0, 512), AF.Exp, scale=0.125)
                            mm(psb(grp["ob"], col0, 512), vm[:, c, h, :], st_["pt"][:, col0:512], start=(c == 0), stop=(c == nch - 1))
                        return (fa, fb)

                    def mknorm(h, qg, grp, odd):
                        def fn():
                            ob = grp["ob"]
                            qsl = slice(qg * 512, (qg + 1) * 512)
                            r_ = rec[ob % 2]
                            if not odd:
                                recip(r_[0:64, :], psb(ob)[64:128, :])
                                tt("dve", yT[0:64, 5 + h // 2, qsl], psb(ob)[0:64, :], r_[0:64, :], ALU.mult)
                            else:
                                recip(r_[64:128, :], psb(ob)[0:64, :])
                                tt("dve", yT[64:128, 5 + h // 2, qsl], psb(ob)[64:128, :], r_[64:128, :], ALU.mult)
                        return (None, fn)
                    for c in range(nch):
                        items.append(mk(h, qg, c, nch, grp))
                    items.append(mknorm(h, qg, grp, odd))
            for f in pipelined(items, 2):
                f()

        def phase5(l, s, SA, xseq, gates, g1bc):
            xsrc = x_d if l == 0 else xs_d
            wo_b = SA.bf(KC * D).rearrange("p (k n) -> p k n", k=KC)
            stg = [SA.f32(D) for _ in range(2)]
            for kc in range(KC):
                dma("sp", stg[kc % 2], wo_d[l, kc * 128:(kc + 1) * 128, :])
                tt("dve", wo_b[:, kc, :], stg[kc % 2], g1bc, ALU.mult)
            xt = [SA.f32(D) for _ in range(2)]
            xhi = [SA.bf(D) for _ in range(2)]
            xlo = [SA.bf(D) for _ in range(2)]
            xTh = [SA.bf(D).rearrange("p (k t) -> p k t", k=KC) for _ in range(2)]
            xTl = [SA.bf(D).rearrange("p (k t) -> p k t", k=KC) for _ in range(2)]
            junk = SA.bf(D)
            st2 = SA.f32(4)
            wr3 = wrT.rearrange("p (k e) -> p k e", e=16)
            wrm = SA.f32(128).rearrange("p (k e) -> p k e", e=16)
            wrh3 = wr_hi.rearrange("p (k e) -> p k e", e=16)
            wrl3 = wr_lo.rearrange("p (k e) -> p k e", e=16)
            tt("dve", wrm, wr3, gm[:, 8:16].unsqueeze(2).to_broadcast([128, KC, 16]), ALU.mult)
            cp("dve", wrh3, wrm)
            tt("dve", wrm, wrm, wrh3, ALU.subtract)
            cp("dve", wrl3, wrm)
            for kc in range(KC):
                mm(psb(7, 0, 16)[0:1, :], sh2[:, kc:kc + 1], wr3[:, kc, :], start=(kc == 0), stop=(kc == KC - 1))
            cp("dve", brow[0:1, :], psb(7, 0, 16)[0:1, :])
            cp("dve", brow_hi[0:1, :], brow[0:1, :])
            tt("dve", brow[0:1, :], brow[0:1, :], brow_hi[0:1, :], ALU.subtract)
            cp("dve", brow_lo[0:1, :], brow[0:1, :])
            plog = psb(6, 0, NT * 16).rearrange("p (i e) -> p i e", e=16)

            def p5(i):
                tsl = slice(i * 128, (i + 1) * 128)
                b0 = (i % 2) * 2
                x_t = xt[i % 2]
                xs_i = xseq[:, i, :]
                ssum = st2[:, (i % 2) * 2:(i % 2) * 2 + 1]
                rstd = st2[:, (i % 2) * 2 + 1:(i % 2) * 2 + 2]
                pbh = psbf(4).rearrange("p (k t) -> p k t", k=KC)
                pbl = psbf(5).rearrange("p (k t) -> p k t", k=KC)

                def s1():
                    for nh in range(2):
                        for kc in range(KC):
                            mm(psb(b0 + nh), yT[:, kc, tsl], wo_b[:, kc, nh * 512:(nh + 1) * 512], start=(kc == 0), stop=(kc == KC - 1))
                    dma("sp", x_t, xsrc[s, i * 128:(i + 1) * 128, :])
                    for nh in range(2):
                        tt("dve", xs_i[:, nh * 512:(nh + 1) * 512], psb(b0 + nh), x_t[:, nh * 512:(nh + 1) * 512], ALU.add)

                def s2():
                    rms_rstd(xs_i, junk, ssum, rstd)
                    act(xhi[i % 2], xs_i, AF.Identity, scale=rstd)
                    stt(xlo[i % 2], xs_i, rstd, xhi[i % 2], ALU.mult, ALU.subtract)

                def s3():
                    for kc in range(KC):
                        tr(pbh[:, kc, :], xhi[i % 2][:, kc * 128:(kc + 1) * 128], ident_b)
                    for kc in range(KC):
                        tr(pbl[:, kc, :], xlo[i % 2][:, kc * 128:(kc + 1) * 128], ident_b)

                def s4():
                    e1 = "act" if i % 2 == 0 else "dve"
                    e2 = "dve" if i % 2 == 0 else "act"
                    cp(e1, xTh[i % 2], pbh)
                    cp(e2, xTl[i % 2], pbl)
                    for kc in range(KC):
                        if i % 2 == 0:
                            act(hT[:, kc, tsl], pbh[:, kc, :], AF.Identity, scale=gm[:, 8 + kc:9 + kc], bias=sh2[:, kc:kc + 1])
                        else:
                            ts("dve", hT[:, kc, tsl], pbh[:, kc, :], gm[:, 8 + kc:9 + kc], sh2[:, kc:kc + 1], ALU.mult, ALU.add)

                def s5():
                    for kc in range(KC):
                        mm(plog[:, i, :], xTh[i % 2][:, kc, :], wrh3[:, kc, :], start=(kc == 0), stop=False)
                        mm(plog[:, i, :], xTl[i % 2][:, kc, :], wrh3[:, kc, :], start=False, stop=False)
                        mm(plog[:, i, :], xTh[i % 2][:, kc, :], wrl3[:, kc, :], start=False, stop=False)
                    mm(plog[:, i, :], ones_b[0:1, :], brow_hi[0:1, :], start=False, stop=False)
                    mm(plog[:, i, :], ones_b[0:1, :], brow_lo[0:1, :], start=False, stop=True)
                return [s1, s2, s3, s4, s5]
            stages = [p5(i) for i in range(NT)]
            delays = [0, 0, 1, 1, 2]
            for t in range(NT + max(delays)):
                for j, dly in enumerate(delays):
                    k = t - dly
                    if 0 <= k < NT:
                        stages[k][j]()
            aff = SA.f32(NT * 16).rearrange("p (i e) -> p i e", e=16)
            bia = SA.f32(NT * 16).rearrange("p (i e) -> p i e", e=16)
            t16 = SA.f32(NT * 16).rearrange("p (i e) -> p i e", e=16)
            m1 = SA.f32(NT * 4)
            m2 = SA.f32(NT * 4)
            gs = SA.f32(NT * 4)
            gmx = SA.f32(NT)
            ing = SA.f32(NT * 4)
            ssm = SA.f32(NT)
            act(aff, plog, AF.Sigmoid)
            tt("dve", bia, aff, brt_bc.unsqueeze(1).to_broadcast([128, NT, 16]), ALU.add)
            bia4 = bia.rearrange("p i (g k) -> p (i g) k", g=4)
            t4 = t16.rearrange("p i (g k) -> p (i g) k", g=4)
            red(m1, bia4, ALU.max)
            tt("dve", t4, bia4, m1.unsqueeze(2).to_broadcast([128, NT * 4, 4]), ALU.is_equal)
            stt(t4, t4, -1.0e9, bia4, ALU.mult, ALU.add)
            red(m2, t4, ALU.max)
            tt("dve", gs, m1, m2, ALU.add)
            red(gmx, gs.rearrange("p (i g) -> p i g", g=4), ALU.max)
            tt("dve", ing.rearrange("p (i g) -> p i g", g=4), gs.rearrange("p (i g) -> p i g", g=4), gmx.unsqueeze(2).to_broadcast([128, NT, 4]), ALU.is_ge)
            tt("dve", t4, bia4, m2.unsqueeze(2).to_broadcast([128, NT * 4, 4]), ALU.is_ge)
            tt("dve", t4, t4, ing.unsqueeze(2).to_broadcast([128, NT * 4, 4]), ALU.mult)
            tt("dve", t16, t16, aff, ALU.mult)
            red(ssm, t16, ALU.add)
            recip(ssm, ssm)
            tt("dve", gates, t16, ssm.unsqueeze(2).to_broadcast([128, NT, 16]), ALU.mult)

        def phase6(l, s, SA, SB_, xseq, gates, g2bc):
            wg32 = [SA.f32(KC * DEXP).rearrange("p (k f) -> p k f", k=KC) for _ in range(2)]
            wu32 = [SA.f32(KC * DEXP).rearrange("p (k f) -> p k f", k=KC) for _ in range(2)]
            wd321 = SA.f32(2 * D).rearrange("p (c n) -> p c n", c=2)
            wd32 = [wd321, wd321]
            wgu = [SB_.bf(KC * 512).rearrange("p (k f) -> p k f", k=KC) for _ in range(2)]
            wdb = [SB_.bf(2 * D).rearrange("p (c n) -> p c n", c=2) for _ in range(2)]
            sgt = [SA.f32(512) for _ in range(2)]
            aT = [SB_.bf(2 * 512).rearrange("p (c t) -> p c t", c=2) for _ in range(2)]
            st6 = {"ndn": 0}
            items = []
            for e in range(NEXP):
                for tg in range(4):
                    def mk(e, tg):
                        p_ = e % 2
                        tsl = slice(tg * 512, (tg + 1) * 512)
                        a_ = aT[tg % 2]

                        def fa():
                            if tg == 0:
                                dma("sp", wg32[p_], wg_d[l, e].rearrange("(k p) f -> p k f", p=128))
                                dma("sp", wu32[p_], wu_d[l, e].rearrange("(k p) f -> p k f", p=128))
                                dma("sp", wd32[p_], wd_d[l, e].rearrange("(c p) n -> p c n", p=128))
                                cp("pool", wgu[p_][:, :, 0:256], wg32[p_])
                                cp("pool", wgu[p_][:, :, 256:512], wu32[p_])
                                tt("pool", wdb[p_], wd32[p_], g2bc.unsqueeze(1).to_broadcast([128, 2, D]), ALU.mult)
                            for half in range(2):
                                bg = half * 2
                                bu = half * 2 + 1
                                for kc in range(KC):
                                    mm(psb(bg), wgu[p_][:, kc, half * 128:(half + 1) * 128], hT[:, kc, tsl], start=(kc == 0), stop=(kc == KC - 1))
                                for kc in range(KC):
                                    mm(psb(bu), wgu[p_][:, kc, 256 + half * 128:256 + (half + 1) * 128], hT[:, kc, tsl], start=(kc == 0), stop=(kc == KC - 1))
                                act(sgt[half], psb(bg), AF.Silu)
                                tt("dve", a_[:, half, :], psb(bu), sgt[half], ALU.mult)

                        def fb():
                            for tl in range(4):
                                i = tg * 4 + tl
                                for nh in range(2):
                                    bank = 4 + (st6["ndn"] % 4)
                                    st6["ndn"] += 1
                                    for half in range(2):
                                        mm(psb(bank), a_[:, half, tl * 128:(tl + 1) * 128], wdb[p_][:, half, nh * 512:(nh + 1) * 512], start=(half == 0), stop=(half == 1))
                                    stt(xseq[:, i, nh * 512:(nh + 1) * 512], psb(bank), gates[:, i, e:e + 1], xseq[:, i, nh * 512:(nh + 1) * 512], ALU.mult, ALU.add)
                        return (fa, fb)
                    items.append(mk(e, tg))
            for f in pipelined(items, 1):
                f()

        def phase7(l, s, SA, xseq):
            if l < DEPTH - 1:
                dma("sp", xs_d[s].rearrange("(i p) d -> p i d", p=128), xseq)
                return
            gfb = SA.f32(D)
            dma("sp", gfb, gfin_d.partition_broadcast(128))
            junk = SA.bf(D)
            st2 = SA.f32(4)
            ot = [SA.f32(D) for _ in range(2)]
            for i in range(NT):
                ssum = st2[:, (i % 2) * 2:(i % 2) * 2 + 1]
                rstd = st2[:, (i % 2) * 2 + 1:(i % 2) * 2 + 2]
                rms_rstd(xseq[:, i, :], junk, ssum, rstd)
                stt(ot[i % 2], xseq[:, i, :], rstd, gfb, ALU.mult, ALU.mult)
                finals.append(dma("sp", out_d[s, i * 128:(i + 1) * 128, :], ot[i % 2]))

        def dump_bf(name, ap2d, n):
            if name in dbg_d:
                o = AW - n
                tmpf = sb[:, o:o + n]
                cp("dve", tmpf, ap2d)
                finals.append(dma("sp", dbg_d[name], tmpf))

        done = False
        if dbg:
            memset("pool", yT.rearrange("p k t -> p (k t)"), 0.0)
        if stop_after == "pro":
            layers = 0
        for l in range(layers):
            for s in range(2):
                first = (l == 0 and s == 0)
                load_vectors(l, s)
                if stop_after == "lv":
                    finals.append(dma("sp", dbg_d["hT"][:, 0:128], vecT))
                    done = True
                    break
                if stop_after == "p1":
                    phase1(l, s, Alloc(S0, AW))
                    if first:
                        dump_bf("hT", hT.rearrange("p k t -> p (k t)"), KC * L)
                    done = True
                    break
                SA12 = Alloc(S0, AW)
                p1cl = phase1(l, s, SA12, emit=False)
                phase2(l, s, SA12, p1cl)
                if first and stop_after in ("p2", "p3", "p4"):
                    dump_bf("hT", hT.rearrange("p k t -> p (k t)"), KC * L)
                if stop_after == "p2":
                    if first:
                        dump_bf("yT", yT.rearrange("p k t -> p (k t)"), KC * L)
                    done = True
                    break
                phase3(l, s, Alloc(S0, AW))
                if stop_after == "p3":
                    if first:
                        dump_bf("yT", yT.rearrange("p k t -> p (k t)"), KC * L)
                    done = True
                    break
                phase4(l, s, Alloc(S0, AW))
                if first and stop_after == "p4":
                    dump_bf("yT", yT.rearrange("p k t -> p (k t)"), KC * L)
                    done = True
                    break
                SA = Alloc(S0, AW)
                xseq = SA.f32(NT * D).rearrange("p (i d) -> p i d", i=NT)
                gates = SA.f32(NT * 16).rearrange("p (i e) -> p i e", e=16)
                gbc_ = [SA.f32(D), SA.f32(D)]
                dma("sp", gbc_[0], mod_d[l, s, 2 * D:3 * D].partition_broadcast(128))
                dma("sp", gbc_[1], mod_d[l, s, 5 * D:6 * D].partition_broadcast(128))
                m5 = SA.o
                phase5(l, s, SA, xseq, gates, gbc_[0])
                if first and "xmid" in dbg_d:
                    finals.append(dma("sp", dbg_d["xmid"].rearrange("(i p) d -> p i d", p=128), xseq))
                if first and "gates" in dbg_d:
                    finals.append(dma("sp", dbg_d["gates"], gates.rearrange("p i e -> p (i e)")))
                if first and "h2T" in dbg_d and stop_after == "p5":
                    dump_bf("h2T", hT.rearrange("p k t -> p (k t)"), KC * L)
                if stop_after == "p5":
                    done = True
                    break
                yo = region(yT)[1] // 4
                phase6(l, s, Alloc(m5, AW), Alloc(yo, yo + KC * L // 2), xseq, gates, gbc_[1])
                if first and "x1" in dbg_d:
                    finals.append(dma("sp", dbg_d["x1"].rearrange("(i p) d -> p i d", p=128), xseq))
                if stop_after == "p6":
                    done = True
                    break
                phase7(l, s, Alloc(m5, AW), xseq)
            if done:
                break
        print("S0 words", S0, "ops", len(P.ops))
        P.emit(nc, final_wait_ops=finals)
    return nc


_CACHE = {}


def kernel(**inputs):
    ctab = make_ctab()
    kind = make_kind()
    if "nc" not in _CACHE:
        _CACHE["nc"] = build_program()
    nc = _CACHE["nc"]
    in_maps = []
    shared = {k: np.ascontiguousarray(np.asarray(v, dtype=np.float32)) for k, v in inputs.items() if k not in ("x", "c")}
    x = np.asarray(inputs["x"], dtype=np.float32)
    c = np.asarray(inputs["c"], dtype=np.float32)
    for core in range(NCORES):
        m = dict(shared)
        m["x"] = np.ascontiguousarray(x[2 * core:2 * core + 2])
        m["c"] = np.ascontiguousarray(c[2 * core:2 * core + 2])
        m["ctab"] = ctab
        m["kind"] = kind
        in_maps.append(m)
    res = run_bass_kernel_spmd(nc, in_maps, core_ids=list(range(NCORES)))
    out = np.concatenate([np.asarray(r["out"]) for r in res.results], axis=0)
    return out.astype(np.float32)
```

```python
import contextlib
import math
import numpy as np
import ml_dtypes
import concourse.bass as bass
import concourse.mybir as mybir
from concourse.bass_utils import run_bass_kernel_spmd

F32 = mybir.dt.float32
BF16 = mybir.dt.bfloat16
ALU = mybir.AluOpType
AF = mybir.ActivationFunctionType
AX = mybir.AxisListType

NCORES = 8
L = 2048
D = 1024
NT = L // 128
KC = D // 128
DEPTH = 2
IN_W = 2340
NEXP = 16
DEXP = 256
NEG = -30000.0
TOPK = 256
NBIS = 10

COMPUTE = ("pe", "act", "dve", "pool")


class Op:
    __slots__ = ("eng", "fn", "deps", "idx", "signal", "dma", "sem", "semval")

    def __init__(self, eng, fn, dma):
        self.eng = eng
        self.fn = fn
        self.deps = set()
        self.signal = False
        self.dma = dma
        self.sem = None
        self.semval = 0


def region(ap):
    t = ap.tensor
    dims = ap.ap
    off = ap.offset
    kind = type(t).__name__
    if kind.startswith("DRam"):
        ext = 1
        for (st, cnt) in dims:
            ext += (cnt - 1) * abs(st)
        return (t.name, off, off + ext, 0, 1)
    row, npart = dims[0]
    esz = mybir.dt.size(ap.dtype)
    if row == 0:
        row = 1 << 40
    p0 = off // row
    col0 = off % row
    ext = 1
    for (st, cnt) in dims[1:]:
        ext += (cnt - 1) * abs(st)
    lo = col0 * esz
    hi = (col0 + ext) * esz
    if kind.startswith("PSum"):
        return (t.name, (lo // 2048) * 2048, ((hi + 2047) // 2048) * 2048, 0, 128)
    return (t.name, lo, hi, p0, p0 + npart)


class Prog:
    def __init__(self):
        self.ops = []
        self.bufs = {}

    def add(self, eng, fn, reads=(), writes=(), dma=False):
        op = Op(eng, fn, dma)
        op.idx = len(self.ops)
        ops = self.ops
        ops.append(op)
        for ap in reads:
            name, lo, hi, p0, p1 = region(ap)
            b = self.bufs.setdefault(name, [[], []])
            for (a, c, q0, q1, w) in b[0]:
                if a < hi and lo < c and q0 < p1 and p0 < q1:
                    op.deps.add(w)
            if name == "ps":
                for (a, c, q0, q1, r) in b[1]:
                    if a < hi and lo < c and ops[r].eng != eng:
                        op.deps.add(r)
            if not dma:
                b[1] = [r for r in b[1] if not (ops[r[4]].eng == eng and not ops[r[4]].dma
                                                and lo <= r[0] and r[1] <= hi and p0 <= r[2] and r[3] <= p1)]
            b[1].append((lo, hi, p0, p1, op.idx))
        for ap in writes:
            name, lo, hi, p0, p1 = region(ap)
            b = self.bufs.setdefault(name, [[], []])
            for (a, c, q0, q1, w) in b[0]:
                if a < hi and lo < c and q0 < p1 and p0 < q1:
                    op.deps.add(w)
            for (a, c, q0, q1, r) in b[1]:
                if a < hi and lo < c and q0 < p1 and p0 < q1:
                    op.deps.add(r)
            b[0] = [w for w in b[0] if not (lo <= w[0] and w[1] <= hi and p0 <= w[2] and w[3] <= p1)]
            b[1] = [r for r in b[1] if not (lo <= r[0] and r[1] <= hi and p0 <= r[2] and r[3] <= p1)]
            b[0].append((lo, hi, p0, p1, op.idx))
        op.deps.discard(op.idx)
        return op

    def emit(self, nc, final_wait_ops=()):
        ops = self.ops
        for op in ops:
            for d in op.deps:
                dop = ops[d]
                if dop.eng == "pe" and op.eng == "pe" and not dop.dma and not op.dma:
                    continue
                dop.signal = True
        for op in final_wait_ops:
            op.signal = True
        for op in ops:
            if op.dma:
                op.signal = True
        engs = {"pe": nc.tensor, "act": nc.scalar, "dve": nc.vector, "pool": nc.gpsimd, "sp": nc.sync}
        with contextlib.ExitStack() as st:
            csem = {e: st.enter_context(nc.semaphore("s_" + e)) for e in COMPUTE}
            ccount = {e: 0 for e in COMPUTE}
            NDMA = 32
            NPOOL = 8
            dsem = [st.enter_context(nc.semaphore("d%d" % i)) for i in range(NDMA)]
            dcount = [0] * NDMA
            dlast = [None] * NDMA
            rr = {"pool": 0, "hw": 0}
            for op in ops:
                if op.dma:
                    if op.signal:
                        if op.eng == "pool":
                            k = rr["pool"] % NPOOL
                            rr["pool"] += 1
                        else:
                            k = NPOOL + rr["hw"] % (NDMA - NPOOL)
                            rr["hw"] += 1
                        if dlast[k] is not None:
                            op.deps.add(dlast[k].idx)
                        dcount[k] += 16
                        op.sem = dsem[k]
                        op.semval = dcount[k]
                        dlast[k] = op
                elif op.signal:
                    ccount[op.eng] += 1
                    op.sem = csem[op.eng]
                    op.semval = ccount[op.eng]
            per_eng = {e: [] for e in engs}
            for op in ops:
                per_eng[op.eng].append(op)
            block = st.enter_context(nc.Block())

            def run_stream(ename, eobj):
                seen = {}
                for op in per_eng[ename]:
                    need = {}
                    for d in op.deps:
                        dop = ops[d]
                        if dop.sem is None:
                            continue
                        if dop.eng == "pe" and ename == "pe" and not dop.dma and not op.dma:
                            continue
                        key = id(dop.sem)
                        if key not in need or need[key][1] < dop.semval:
                            need[key] = (dop.sem, dop.semval)
                    for key, (sem, val) in need.items():
                        if seen.get(key, 0) >= val:
                            continue
                        eobj.wait_ge(sem, val)
                        seen[key] = val
                    ins = op.fn(eobj)
                    if op.sem is not None:
                        ins.then_inc(op.sem, 16 if op.dma else 1)
                if ename == "sp":
                    for k in range(NDMA):
                        if dcount[k] > 0:
                            eobj.wait_ge(dsem[k], dcount[k])

            @block.tensor
            def _(e):
                run_stream("pe", e)

            @block.scalar
            def _(e):
                run_stream("act", e)

            @block.vector
            def _(e):
                run_stream("dve", e)

            @block.gpsimd
            def _(e):
                run_stream("pool", e)

            @block.sync
            def _(e):
                run_stream("sp", e)


CT_ID = 0
CT_TRINEG = 128
CT_TRIT = 256
CT_NEGI = 384
CT_CS64 = 512
CT_CS32 = 768
CT_PADC = 896
CT_NEGM = 1024
CT_POW2 = 1152
CT_OH = 1184
CT_W = 1184 + 2048


def make_ctab():
    t = np.zeros((128, CT_W), np.float32)
    p = np.arange(128)
    t[:, CT_ID:CT_ID + 128] = np.eye(128)
    t[:, CT_TRINEG:CT_TRINEG + 128] = np.where(p[None, :] <= p[:, None], 0.0, -3.0e38)
    t[:, CT_TRIT:CT_TRIT + 128] = np.where(p[:, None] <= p[None, :], 0.0, NEG)
    t[:, CT_NEGI:CT_NEGI + 128] = NEG * np.eye(128)
    pos = (np.arange(NT)[None, :] * 128 + p[:, None]).astype(np.float32)
    for (base, rot) in ((CT_CS64, 16), (CT_CS32, 8)):
        half = rot // 2
        inv = np.exp(np.float32(-math.log(500000.0)) * (np.arange(half, dtype=np.float32) * np.float32(2.0) / np.float32(rot))).astype(np.float32)
        ang = (pos[:, :, None] * inv[None, None, :]).astype(np.float32)
        cs = np.concatenate([np.cos(ang), np.sin(ang)], axis=-1).astype(np.float32)
        t[:, base:base + NT * rot] = cs.reshape(128, NT * rot)
    own = np.arange(NT) // 2
    n = np.arange(8)
    t[:, CT_PADC:CT_PADC + 128] = np.where(n[None, :] < own[:, None], 0.0, -1.0e30).reshape(1, 128)
    t[:, CT_NEGM:CT_NEGM + 128] = np.where(n[None, :] < own[:, None], NEG, 0.0).reshape(1, 128)
    t[:, CT_POW2:CT_POW2 + 32] = (2.0 ** -np.arange(32))[None, :]
    oh = np.zeros((128, 16, 128), np.float32)
    for e in range(16):
        oh[e, e, :] = 1.0
    t[:, CT_OH:CT_OH + 2048] = oh.reshape(128, 2048)
    return t


def make_kind():
    k = np.zeros((8, L), np.float32)
    for n in range(8):
        k[n, n * 256:(n + 1) * 256] = 1.0
    return k


def build_program(dbg=None, layers=DEPTH, stop_after=None):
    dbg = dbg or {}
    nc = bass.Bass("TRN2", target_bir_lowering=False)

    def din(name, shape):
        return nc.dram_tensor(name, list(shape), F32, kind="ExternalInput").ap()

    x_d = din("x", [2, L, D])
    c_d = din("c", [2, D])
    wada_d = din("w_ada", [DEPTH, D, 6 * D])
    bada_d = din("b_ada", [DEPTH, 6 * D])
    gmix_d = din("g_mix", [DEPTH, D])
    win_d = din("w_in", [DEPTH, D, IN_W])
    wdw_d = din("w_dw", [DEPTH, 31, 256])
    bdw_d = din("b_dw", [DEPTH, 256])
    lng_d = din("ln_conv_g", [DEPTH, 256])
    lnb_d = din("ln_conv_b", [DEPTH, 256])
    wo_d = din("w_o", [DEPTH, D, D])
    gffn_d = din("g_ffn", [DEPTH, D])
    wr_d = din("w_router", [D, NEXP])
    br_d = din("b_router", [NEXP])
    wg_d = din("w_gate", [DEPTH, NEXP, D, DEXP])
    wu_d = din("w_up", [DEPTH, NEXP, D, DEXP])
    wd_d = din("w_down", [DEPTH, NEXP, DEXP, D])
    gfin_d = din("g_final", [D])
    ctab_d = din("ctab", [128, CT_W])
    kind_d = din("kind", [8, L])
    out_d = nc.dram_tensor("out", [2, L, D], F32, kind="ExternalOutput").ap()
    xs_d = nc.dram_tensor("xs_scr", [2, L, D], F32).ap()
    mod_d = nc.dram_tensor("mod_scr", [DEPTH, 2, 6 * D], F32).ap()
    dbg_d = {k: nc.dram_tensor("dbg_" + k, list(shp), F32, kind="ExternalOutput").ap() for k, shp in dbg.items()}

    P = Prog()
    finals = []

    with contextlib.ExitStack() as st:
        AW = 53000
        sb = st.enter_context(nc.sbuf_tensor("arena", [128, AW], F32))
        ps = st.enter_context(nc.psum_tensor("ps", [128, 8, 512], F32))

        class Alloc:
            def __init__(self, base, limit):
                self.o = base
                self.limit = limit

            def f32(self, n):
                o = self.o
                self.o += n
                assert self.o <= self.limit, (self.o, self.limit)
                return sb[:, o:o + n]

            def bf(self, n):
                w = (n + 1) // 2
                o = self.o
                self.o += w
                assert self.o <= self.limit, (self.o, self.limit)
                return sb[:, o:o + w].bitcast(BF16)

        def aps(*xs):
            return [x for x in xs if x is not None and not isinstance(x, (int, float))]

        def mm(out, lhsT, rhs, start=True, stop=True):
            P.add("pe", lambda e: e.matmul(out, lhsT=lhsT, rhs=rhs, start=start, stop=stop), reads=[lhsT, rhs], writes=[out])

        def tr(out, in_, ident):
            P.add("pe", lambda e: e.transpose(out=out, in_=in_, identity=ident), reads=[in_, ident], writes=[out])

        def act(out, in_, func, scale=1.0, bias=None, accum=None):
            kw = {}
            if bias is not None:
                kw["bias"] = bias
            if accum is not None:
                kw["accum_out"] = accum
            P.add("act", lambda e: e.activation(out=out, in_=in_, func=func, scale=scale, **kw),
                  reads=aps(in_, scale, bias), writes=aps(out, accum))

        def ts(eng, out, in0, s1, s2=None, op0=ALU.mult, op1=None, accum=None):
            kw = {}
            if op1 is not None:
                kw["op1"] = op1
            if accum is not None:
                kw["accum_out"] = accum
            P.add(eng, lambda e: e.tensor_scalar(out=out, in0=in0, scalar1=s1, scalar2=s2, op0=op0, **kw),
                  reads=aps(in0, s1, s2), writes=aps(out, accum))

        def tt(eng, out, in0, in1, op):
            P.add(eng, lambda e: e.tensor_tensor(out=out, in0=in0, in1=in1, op=op), reads=[in0, in1], writes=[out])

        def stt(out, in0, scalar, in1, op0, op1):
            P.add("dve", lambda e: e.scalar_tensor_tensor(out=out, in0=in0, scalar=scalar, in1=in1, op0=op0, op1=op1),
                  reads=aps(in0, scalar, in1), writes=[out])

        def cp(eng, out, in_):
            if eng == "act":
                P.add("act", lambda e: e.activation(out=out, in_=in_, func=AF.Copy), reads=[in_], writes=[out])
            else:
                P.add(eng, lambda e: e.tensor_copy(out=out, in_=in_), reads=[in_], writes=[out])

        def memset(eng, out, val):
            P.add(eng, lambda e: e.memset(out, val), writes=[out])

        def dma(eng, out, in_):
            return P.add(eng, lambda e: e.dma_start(out=out, in_=in_), reads=[in_], writes=[out], dma=True)

        def red(out, in_, op, axis=AX.X):
            P.add("dve", lambda e: e.tensor_reduce(out=out, in_=in_, axis=axis, op=op), reads=[in_], writes=[out])

        def recip(out, in_):
            P.add("dve", lambda e: e.reciprocal(out=out, in_=in_), reads=[in_], writes=[out])

        def max8(out, in_):
            P.add("dve", lambda e: e.max(out=out, in_=in_), reads=[in_], writes=[out])

        def dump(name, ap_sb, dram_ap=None):
            if name in dbg_d:
                finals.append(dma("sp", dram_ap if dram_ap is not None else dbg_d[name], ap_sb))

        def psb(b, lo=0, hi=512):
            return ps[:, b, lo:hi]

        def psbf(b):
            return ps[:, b, :].bitcast(BF16)

        A = Alloc(0, AW)
        ctab = A.f32(CT_OH)
        ident_f = ctab[:, CT_ID:CT_ID + 128]
        trineg = ctab[:, CT_TRINEG:CT_TRINEG + 128]
        cs64 = ctab[:, CT_CS64:CT_CS64 + 256].rearrange("p (i r) -> p i r", r=16)
        cs32 = ctab[:, CT_CS32:CT_CS32 + 128].rearrange("p (i r) -> p i r", r=8)
        padc = ctab[:, CT_PADC:CT_PADC + 128].rearrange("p (i n) -> p i n", n=8)
        negm = ctab[:, CT_NEGM:CT_NEGM + 128].rearrange("p (i n) -> p i n", n=8)
        pow2 = ctab[:, CT_POW2:CT_POW2 + 32]
        ident_b = A.bf(128)
        trit_b = A.bf(128)
        negi3 = A.bf(384).rearrange("p (g q) -> p g q", g=3)
        onesq_b = A.bf(128)
        ones_b = A.bf(128)
        oh_b = A.bf(2048).rearrange("p (e m) -> p e m", e=16)
        cst = A.f32(8)
        rows = A.f32(128)
        vecT = A.f32(128)
        gm = A.f32(16)
        brt_bc = A.f32(16)
        wrT = A.f32(128)
        wr_hi = A.bf(128)
        wr_lo = A.bf(128)
        brow = A.f32(16)
        brow_hi = A.bf(16)
        brow_lo = A.bf(16)
        cactT = A.bf(16)
        sh2col = A.bf(8)
        hT = A.bf(KC * L).rearrange("p (k t) -> p k t", k=KC)
        yT = A.bf(KC * L).rearrange("p (k t) -> p k t", k=KC)
        S0 = A.o

        dma("sp", ctab, ctab_d[:, 0:CT_OH])
        cp("pool", ident_b, ident_f)
        cp("pool", trit_b, ctab[:, CT_TRIT:CT_TRIT + 128])
        for g in range(3):
            cp("pool", negi3[:, g, :], ctab[:, CT_NEGI:CT_NEGI + 128])
        memset("pool", onesq_b, 1.0 / 256.0)
        memset("pool", ones_b, 1.0)
        ohf = sb[:, S0:S0 + 2048]
        dma("sp", ohf, ctab_d[:, CT_OH:CT_OH + 2048])
        cp("pool", oh_b.rearrange("p e m -> p (e m)"), ohf)
        memset("pool", cst[:, 0:1], 1e-6)
        memset("pool", rows, 0.0)
        dma("sp", brt_bc, br_d.partition_broadcast(128))
        for kc in range(KC):
            dma("sp", wrT[:, kc * 16:(kc + 1) * 16], wr_d[kc * 128:(kc + 1) * 128, :])

        PA = Alloc(S0 + 2048, AW)
        cin = PA.f32(128)
        dma("sp", cin[0:16, :], c_d.rearrange("b (k p) -> (b k) p", p=128))
        act(cin[0:16, :], cin[0:16, :], AF.Silu)
        tr(psb(0, 0, 16), cin[0:16, :], ident_f[0:16, 0:16])
        cp("dve", cactT, psb(0, 0, 16))
        cactT3 = cactT.rearrange("p (b k) -> p b k", b=2)
        cbc = PA.bf(KC * 128).rearrange("p (k m) -> p k m", k=KC)
        for b in range(2):
            cp("dve", cbc[:, :, b * 64:(b + 1) * 64], cactT3[:, b, :].unsqueeze(2).to_broadcast([128, KC, 64]))
        wada_b = PA.bf(KC * 1536).rearrange("p (k n) -> p k n", k=KC)
        bada = PA.f32(6 * D)
        modrow = PA.f32(6 * D)
        import os
        PRO = os.environ.get("PRO", "all")
        for l in range(DEPTH if PRO != "nowada" else 0):
            dma("sp", bada, bada_d[l].partition_broadcast(128))
            for q in range(4):
                for kc in range(KC):
                    dma("pool", wada_b[:, kc, :], wada_d[l, kc * 128:(kc + 1) * 128, q * 1536:(q + 1) * 1536])
                for j in range(3):
                    bank = (q * 3 + j) % 2
                    c0 = q * 1536 + j * 512
                    for kc in range(KC):
                        mm(psb(bank), cbc[:, kc, :], wada_b[:, kc, j * 512:(j + 1) * 512], start=(kc == 0), stop=(kc == KC - 1))
                    tt("dve", modrow[:, c0:c0 + 512], psb(bank), bada[:, c0:c0 + 512], ALU.add)
            dma("sp", mod_d[l, 0:1, :], modrow[0:1, :])
            dma("sp", mod_d[l, 1:2, :], modrow[64:65, :])
            if l == 0 and "mod0" in dbg_d:
                finals.append(dma("sp", dbg_d["mod0"][0:1, :], modrow[0:1, :]))
                finals.append(dma("sp", dbg_d["mod0"][1:2, :], modrow[64:65, :]))

        def load_vectors(l, s):
            srcs = [mod_d[l, s, 0:D], mod_d[l, s, D:2 * D], mod_d[l, s, 3 * D:4 * D], mod_d[l, s, 4 * D:5 * D],
                    gmix_d[l], gffn_d[l]]
            for j, src in enumerate(srcs):
                dma("sp", rows[8 * j:8 * j + 8, :], src.rearrange("(c p) -> c p", p=128))
            dma("sp", rows[48:79, :], wdw_d[l, :, 0:128])
            dma("sp", rows[79:110, :], wdw_d[l, :, 128:256])
            dma("sp", rows[110:112, :], bdw_d[l].rearrange("(c p) -> c p", p=128))
            dma("sp", rows[112:114, :], lng_d[l].rearrange("(c p) -> c p", p=128))
            dma("sp", rows[114:116, :], lnb_d[l].rearrange("(c p) -> c p", p=128))
            tr(psb(7, 0, 128), rows, ident_f)
            cp("dve", vecT, psb(7, 0, 128))
            stt(gm[:, 0:8], vecT[:, 8:16], 1.0, vecT[:, 32:40], ALU.add, ALU.mult)
            stt(gm[:, 8:16], vecT[:, 24:32], 1.0, vecT[:, 40:48], ALU.add, ALU.mult)

        sh1 = vecT[:, 0:8]
        sh2 = vecT[:, 16:24]
        wdwT = [vecT[:, 48:79], vecT[:, 79:110]]
        bdwT = vecT[:, 110:112]
        lngT = vecT[:, 112:114]
        lnbT = vecT[:, 114:116]

        def rms_rstd(xt, junk, ssum, rstd):
            act(junk, xt, AF.Square, accum=ssum)
            act(ssum, ssum, AF.Sqrt, scale=1.0 / D, bias=cst[:, 0:1])
            recip(rstd, ssum)

        def phase1(l, s, SA):
            xsrc = x_d if l == 0 else xs_d
            xt = [SA.f32(D) for _ in range(2)]
            xn = [SA.bf(D) for _ in range(2)]
            junk = SA.bf(D)
            st2 = SA.f32(4)
            def p1(i):
                x_t = xt[i % 2]
                ssum = st2[:, (i % 2) * 2:(i % 2) * 2 + 1]
                rstd = st2[:, (i % 2) * 2 + 1:(i % 2) * 2 + 2]

                def fa():
                    dma("sp", x_t, xsrc[s, i * 128:(i + 1) * 128, :])
                    rms_rstd(x_t, junk, ssum, rstd)
                    act(xn[i % 2], x_t, AF.Identity, scale=rstd)

                def fb():
                    pb = psbf(i % 2).rearrange("p (k t) -> p k t", k=KC)
                    for kc in range(KC):
                        tr(pb[:, kc, :], xn[i % 2][:, kc * 128:(kc + 1) * 128], ident_b)
                    for kc in range(KC):
                        if i % 2 == 0:
                            act(hT[:, kc, i * 128:(i + 1) * 128], pb[:, kc, :], AF.Identity, scale=gm[:, kc:kc + 1], bias=sh1[:, kc:kc + 1])
                        else:
                            ts("dve", hT[:, kc, i * 128:(i + 1) * 128], pb[:, kc, :], gm[:, kc:kc + 1], sh1[:, kc:kc + 1], ALU.mult, ALU.add)
                return (fa, fb)
            for f in pipelined([p1(i) for i in range(NT)], 1):
                f()

        def load_win(l, c0, c1, SA):
            w = SA.bf(KC * (c1 - c0)).rearrange("p (k n) -> p k n", k=KC)
            for kc in range(KC):
                dma("pool", w[:, kc, :], win_d[l, kc * 128:(kc + 1) * 128, c0:c1])
            return w

        def phase2(l, s, SA, w=None):
            if w is None:
                w = load_win(l, 0, 512, SA)
            apad = SA.bf(2 * (L + 32)).rearrange("p (c t) -> p c t", c=2)
            dg = SA.bf(2 * 31 * 128).rearrange("p (c j m) -> p c j m", c=2, j=31)
            acc = SA.f32(2 * L).rearrange("p (c t) -> p c t", c=2)
            sg = [SA.f32(512) for _ in range(2)]
            ybf = SA.bf(2 * 512).rearrange("p (c t) -> p c t", c=2)
            ysq = SA.bf(2 * 512).rearrange("p (c t) -> p c t", c=2)
            tmp = SA.f32(512)
            rstd = SA.f32(512)
            dd = [SA.f32(512) for _ in range(2)]
            memset("pool", apad[:, :, 0:32], 0.0)
            for cc in range(2):
                for j in range(31):
                    ts("dve" if j % 2 == 0 else "pool", dg[:, cc, j, :], ident_f, wdwT[cc][:, j:j + 1], 1.0, ALU.mult, ALU.mult)
            for tg in range(4):
                tsl = slice(tg * 512, (tg + 1) * 512)
                for cc in range(2):
                    for half, col in ((0, cc * 128), (1, 256 + cc * 128)):
                        bank = cc * 2 + half
                        for kc in range(KC):
                            mm(psb(bank), w[:, kc, col:col + 128], hT[:, kc, tsl], start=(kc == 0), stop=(kc == KC - 1))
                    act(sg[cc], psb(cc * 2 + 1), AF.Sigmoid)
                    tt("dve", apad[:, cc, 32 + tg * 512:32 + (tg + 1) * 512], psb(cc * 2), sg[cc], ALU.mult)
            for tg in range(4):
                tsl = slice(tg * 512, (tg + 1) * 512)
                for cc in range(2):
                    for j in range(31):
                        o = 2 + j + tg * 512
                        mm(psb(4 + cc), dg[:, cc, j, :], apad[:, cc, o:o + 512], start=(j == 0), stop=(j == 30))
                    act(acc[:, cc, tsl], psb(4 + cc), AF.Identity, bias=bdwT[:, cc:cc + 1])
                    cp("dve", ybf[:, cc, :], acc[:, cc, tsl])
                    act(ysq[:, cc, :], acc[:, cc, tsl], AF.Square)
                for cc in range(2):
                    mm(psb(6), onesq_b, ybf[:, cc, :], start=(cc == 0), stop=(cc == 1))
                for cc in range(2):
                    mm(psb(7), onesq_b, ysq[:, cc, :], start=(cc == 0), stop=(cc == 1))
                cp("act", tmp, psb(6))
                tt("dve", rstd, tmp, tmp, ALU.mult)
                tt("dve", rstd, psb(7), rstd, ALU.subtract)
                ts("dve", rstd, rstd, 0.0, None, ALU.max)
                act(rstd, rstd, AF.Sqrt, bias=cst[:, 0:1])
                recip(rstd, rstd)
                for cc in range(2):
                    tt("dve", dd[cc], acc[:, cc, tsl], tmp, ALU.subtract)
                    tt("dve", dd[cc], dd[cc], rstd, ALU.mult)
                    act(yT[:, cc, tsl], dd[cc], AF.Silu, scale=lngT[:, cc:cc + 1], bias=lnbT[:, cc:cc + 1])

        def rope(dst, src, nh, hd, half, cs_i, tmp):
            cos = cs_i[:, 0:half].unsqueeze(1).to_broadcast([128, nh, half])
            sin = cs_i[:, half:2 * half].unsqueeze(1).to_broadcast([128, nh, half])
            x1 = src[:, :, 0:half]
            x2 = src[:, :, half:2 * half]
            t1 = tmp[:, 0:nh * half].rearrange("p (h r) -> p h r", h=nh)
            t2 = tmp[:, 64:64 + nh * half].rearrange("p (h r) -> p h r", h=nh)
            t3 = tmp[:, 128:128 + nh * half].rearrange("p (h r) -> p h r", h=nh)
            t4 = tmp[:, 192:192 + nh * half].rearrange("p (h r) -> p h r", h=nh)
            tt("dve", t1, x1, cos, ALU.mult)
            tt("dve", t2, x2, sin, ALU.mult)
            tt("dve", t3, x2, cos, ALU.mult)
            tt("dve", t4, x1, sin, ALU.mult)
            tt("dve", dst[:, :, 0:half], t1, t2, ALU.subtract)
            tt("dve", dst[:, :, half:2 * half], t3, t4, ALU.add)
            cp("act", dst[:, :, 2 * half:hd], src[:, :, 2 * half:hd])

        def pipelined(items, depth):
            out = []
            n = len(items)
            for k in range(n + depth):
                def f(k=k):
                    if k < n and items[k][0] is not None:
                        items[k][0]()
                    if k - depth >= 0 and items[k - depth][1] is not None:
                        items[k - depth][1]()
                out.append(f)
            return out

        def phase3(l, s, SA, w=None):
            qT = SA.bf(6 * L).rearrange("p (h t) -> p h t", h=6)
            kT = SA.bf(L)
            vd = SA.bf(NT * 128).rearrange("p (c m) -> p c m", c=NT)
            vds = SA.bf(NT * 128).rearrange("p (c m) -> p c m", c=NT)
            iqT = SA.bf(4 * L).rearrange("p (h t) -> p h t", h=4)
            ikT = SA.bf(L)
            widx = SA.f32(NT * 4).rearrange("p (i h) -> p i h", h=4)
            mark = SA.o
            if w is None:
                w = load_win(l, 512, 1188, SA)
            qs = [SA.bf(7 * 64).rearrange("p (h d) -> p h d", h=7) for _ in range(2)]
            iqs = [SA.bf(5 * 32).rearrange("p (h d) -> p h d", h=5) for _ in range(2)]
            rtmp = [SA.f32(256) for _ in range(2)]
            rtmp2 = [SA.f32(256) for _ in range(2)]
            memset("pool", vd[:, :, 64:128], 1.0)
            memset("pool", vds[:, :, 0:64], 1.0)
            slot = {0: 0, 2: 1, 4: 2, 1: 3, 3: 4, 5: 5}
            def p3a(i):
                tsl = slice(i * 128, (i + 1) * 128)
                b0 = (i % 2) * 2

                def fa():
                    for (bank, c0, c1) in ((b0, 0, 512), (b0 + 1, 512, 676)):
                        for kc in range(KC):
                            mm(psb(bank, 0, c1 - c0), hT[:, kc, tsl], w[:, kc, c0:c1], start=(kc == 0), stop=(kc == KC - 1))
                    pq = psb(b0, 0, 448).rearrange("p (h d) -> p h d", h=7)
                    rope(qs[i % 2], pq, 7, 64, 8, cs64[:, i, :], rtmp[i % 2])
                    cp("act", vd[:, i, 0:64], psb(b0, 448, 512))
                    cp("dve", vds[:, i, 64:128], psb(b0, 448, 512))
                    piq = psb(b0 + 1, 0, 160).rearrange("p (h d) -> p h d", h=5)
                    rope(iqs[i % 2], piq, 5, 32, 4, cs32[:, i, :], rtmp2[i % 2])
                    cp("act", widx[:, i, :], psb(b0 + 1, 160, 164))

                def fb():
                    pt = psbf(4 + (i % 2))
                    ptq = pt[:, 0:768].rearrange("p (h t) -> p h t", h=6)
                    for h in range(6):
                        tr(ptq[0:64, slot[h], :], qs[i % 2][:, h, :], ident_b)
                    tr(pt[0:64, 768:896], qs[i % 2][:, 6, :], ident_b)
                    cp("act", qT[0:64, :, tsl], ptq[0:64, :, :])
                    cp("dve", kT[0:64, tsl], pt[0:64, 768:896])
                    pt2 = psbf(6 + (i % 2))
                    pti = pt2[:, 0:512].rearrange("p (h t) -> p h t", h=4)
                    for h in range(4):
                        tr(pti[0:32, h, :], iqs[i % 2][:, h, :], ident_b)
                    tr(pt2[0:32, 512:640], iqs[i % 2][:, 4, :], ident_b)
                    cp("act", iqT[0:32, :, tsl], pti[0:32, :, :])
                    cp("dve", ikT[0:32, tsl], pt2[0:32, 512:640])
                return (fa, fb)
            for f in pipelined([p3a(i) for i in range(NT)], 1):
                f()
            SA.o = mark
            score = [SA.f32(L) for _ in range(4)]
            rel = [SA.f32(512) for _ in range(2)]
            junk = [SA.bf(L) for _ in range(2)]
            notsel = [SA.bf(L) for _ in range(4)]
            PT = [SA.bf(768).rearrange("p (g q) -> p g q", g=2) for _ in range(3)]
            bst = [SA.f32(64) for _ in range(4)]
            rec = SA.f32(384)
            otS1 = SA.f32(768).rearrange("p (g q) -> p g q", g=2)
            otS = [otS1, otS1]
            cnt_ = {"npt": 0, "lb": 0}

            def score_chunks(i):
                S = 128 * (i + 1)
                sc = score[i % 4]
                tsl = slice(i * 128, (i + 1) * 128)
                nchunk = (S + 511) // 512
                out = []

                def mk(ch):
                    def f():
                        k0 = ch * 512
                        k1 = min(S, k0 + 512)
                        for h in range(4):
                            bank = cnt_["lb"] % 2
                            cnt_["lb"] += 1
                            mm(psb(bank, 0, k1 - k0), iqT[0:32, h, tsl], ikT[0:32, k0:k1])
                            if h == 0:
                                ts("dve", sc[:, k0:k1], psb(bank, 0, k1 - k0), 0.0, widx[:, i, 0:1], ALU.max, ALU.mult)
                            else:
                                r = rel[h % 2]
                                act(r[:, 0:k1 - k0], psb(bank, 0, k1 - k0), AF.Relu)
                                stt(sc[:, k0:k1], r[:, 0:k1 - k0], widx[:, i, h:h + 1], sc[:, k0:k1], ALU.mult, ALU.add)
                        if ch == nchunk - 1:
                            tt("dve", sc[:, i * 128:S], sc[:, i * 128:S], trineg, ALU.add)
                    return f
                for ch in range(nchunk):
                    out.append(mk(ch))
                return out

            def bisect_init(i):
                def f():
                    sc = score[i % 4]
                    bs = bst[i % 4]
                    pre = sc[:, 0:i * 128]
                    mx = bs[:, 0:1]
                    mn = bs[:, 1:2]
                    cand = bs[:, 2:3]
                    steps = bs[:, 8:8 + NBIS + 1]
                    red(mx, pre, ALU.max)
                    red(mn, pre, ALU.min)
                    tt("dve", mx, mx, mn, ALU.subtract)
                    ts("dve", steps, pow2[:, 1:NBIS + 2], mx, None, ALU.mult)
                    tt("dve", cand, mn, steps[:, 0:1], ALU.add)
                return f

            def bisect_steps(i, engs):
                S = 128 * (i + 1)
                sc = score[i % 4]
                bs = bst[i % 4]
                cand = bs[:, 2:3]
                cnt = bs[:, 3:4]
                dlt = bs[:, 4:5]
                steps = bs[:, 8:8 + NBIS + 1]
                out = []

                def mk(it):
                    def f():
                        if engs[it] == "dve":
                            ts("dve", junk[0][:, 0:S], sc[:, 0:S], cand, None, ALU.is_ge, ALU.add, accum=cnt)
                            ts("dve", dlt, cnt, TOPK - 0.5, 0.5, ALU.is_ge, ALU.subtract)
                        else:
                            act(junk[1][:, 0:S], sc[:, 0:S], AF.Sign, scale=-1.0, bias=cand, accum=cnt)
                            ts("dve", dlt, cnt, S - 2 * TOPK + 1.0, 0.5, ALU.is_le, ALU.subtract)
                        if it == NBIS - 1:
                            ts("dve", dlt, dlt, 0.5, None, ALU.subtract)
                        stt(cand, dlt, steps[:, it:it + 1], cand, ALU.mult, ALU.add)
                    return f
                for it in range(NBIS):
                    out.append(mk(it))
                return out

            def finish(i):
                S = 128 * (i + 1)
                sc = score[i % 4]
                ns = notsel[i % 4]
                if i >= 2:
                    ts("dve", ns[:, 0:S], sc[:, 0:S], bst[i % 4][:, 2:3], None, ALU.is_lt)
                else:
                    ts("dve", ns[:, 0:S], sc[:, 0:S], -1.0e37, None, ALU.is_lt)

            def attention(i):
                tsl = slice(i * 128, (i + 1) * 128)
                ns = notsel[i % 4]
                items = []

                def mk(c):
                    st_ = {}

                    def fa():
                        csl = slice(c * 128, (c + 1) * 128)
                        st_["sb0"] = 2 + 2 * (cnt_["npt"] % 2)
                        st_["pt"] = PT[cnt_["npt"] % 3]
                        cnt_["npt"] += 1
                        for g in range(2):
                            mm(psb(st_["sb0"] + g, 0, 384), kT[0:64, csl], qT[0:64, 3 * g:3 * g + 3, tsl], start=True, stop=False)
                            mm(psb(st_["sb0"] + g, 0, 384), ns[:, csl], negi3, start=False, stop=True)

                    def fb():
                        act(st_["pt"], ps[:, st_["sb0"]:st_["sb0"] + 2, 0:384], AF.Exp, scale=0.125)
                        for g in range(2):
                            mm(psb(6 + g, 0, 384), (vd if g == 0 else vds)[:, c, :], st_["pt"][:, g, :], start=(c == 0), stop=(c == i))
                    return (fa, fb)

                def norm():
                    o_ = otS[i % 2]
                    cp("dve", o_, ps[:, 6:8, 0:384])
                    act(rec[0:64, :], o_[64:128, 0, :], AF.Ln)
                    act(rec[0:64, :], rec[0:64, :], AF.Exp, scale=-1.0)
                    tt("dve", yT[0:64, 2:5, tsl], o_[0:64, 0, :].rearrange("p (h q) -> p h q", h=3), rec[0:64, :].rearrange("p (h q) -> p h q", h=3), ALU.mult)
                    act(rec[64:128, :], o_[0:64, 1, :], AF.Ln)
                    act(rec[64:128, :], rec[64:128, :], AF.Exp, scale=-1.0)
                    tt("dve", yT[64:128, 2:5, tsl], o_[64:128, 1, :].rearrange("p (h q) -> p h q", h=3), rec[64:128, :].rearrange("p (h q) -> p h q", h=3), ALU.mult)
                for c in range(i + 1):
                    items.append(mk(c))
                items.append((None, norm))
                return items

            for f in score_chunks(0) + score_chunks(1):
                f()
            pending = []
            for j in range(NT // 2):
                ta, tb = 2 * j, 2 * j + 1
                sa = bisect_steps(ta, ["act" if it % 2 == 1 else "dve" for it in range(NBIS)]) if ta >= 2 else []
                sb_ = bisect_steps(tb, ["act"] * NBIS) if tb >= 2 else []
                nxt = []
                if j + 1 < NT // 2:
                    nxt = score_chunks(ta + 2) + score_chunks(tb + 2) + [bisect_init(ta + 2), bisect_init(tb + 2)]
                nr = NBIS if sa else 1
                for it in range(nr):
                    if sb_:
                        sb_[it]()
                    if sa:
                        sa[it]()
                    left = nr - it
                    for _ in range((len(pending) + left - 1) // left):
                        pending.pop(0)()
                    for _ in range((len(nxt) + left - 1) // left):
                        nxt.pop(0)()
                while pending:
                    pending.pop(0)()
                while nxt:
                    nxt.pop(0)()
                finish(ta)
                finish(tb)
                pending = pipelined(attention(ta) + attention(tb), 1)
            while pending:
                pending.pop(0)()

        def phase4(l, s, SA):
            w = load_win(l, 1188, 2340, SA)
            qa = SA.bf(6 * L).rearrange("p (h t) -> p h t", h=6)
            ka = SA.bf(6 * L).rearrange("p (h t) -> p h t", h=6)
            vm = SA.bf(NT * 6 * 128).rearrange("p (c h m) -> p c h m", c=NT, h=6)
            kmT = SA.bf(6 * 8).rearrange("p (h n) -> p h n", h=6)
            kms = SA.f32(6 * 8).rearrange("p (h n) -> p h n", h=6)
            qs = [SA.bf(6 * 64).rearrange("p (h d) -> p h d", h=6) for _ in range(2)]
            ks = [SA.bf(6 * 64).rearrange("p (h d) -> p h d", h=6) for _ in range(2)]
            qb = [SA.bf(6 * 72).rearrange("p (h d) -> p h d", h=6) for _ in range(2)]
            rtmp = [SA.f32(256) for _ in range(2)]
            rtmp2 = [SA.f32(256) for _ in range(2)]
            gmk = [SA.f32(48).rearrange("p (h n) -> p h n", h=6) for _ in range(2)]
            g8 = [SA.f32(48).rearrange("p (h n) -> p h n", h=6) for _ in range(2)]
            kindf = SA.f32(L)
            dma("sp", kindf[64:72, :], kind_d)
            for h in range(6):
                cp("pool", ka[64:72, h, :], kindf[64:72, :])
            for par in range(2):
                memset("pool", qb[par], 0.0)
            for h in range(6):
                if h % 2 == 0:
                    memset("pool", vm[:, :, h, 64:128], 1.0)
                else:
                    memset("pool", vm[:, :, h, 0:64], 1.0)
            def p4a(i):
                tsl = slice(i * 128, (i + 1) * 128)
                b0 = (i % 2) * 3

                def fa():
                    for j in range(3):
                        for kc in range(KC):
                            mm(psb(b0 + j, 0, 384), hT[:, kc, tsl], w[:, kc, j * 384:(j + 1) * 384], start=(kc == 0), stop=(kc == KC - 1))
                    pq = psb(b0, 0, 384).rearrange("p (h d) -> p h d", h=6)
                    pk = psb(b0 + 1, 0, 384).rearrange("p (h d) -> p h d", h=6)
                    pv = psb(b0 + 2, 0, 384).rearrange("p (h d) -> p h d", h=6)
                    rope(qs[i % 2], pq, 6, 64, 8, cs64[:, i, :], rtmp[i % 2])
                    rope(ks[i % 2], pk, 6, 64, 8, cs64[:, i, :], rtmp2[i % 2])
                    for h in range(6):
                        o = 0 if h % 2 == 0 else 64
                        cp("act" if h % 2 == 0 else "dve", vm[:, i, h, o:o + 64], pv[:, h, :])

                def fb():
                    ptq = psbf(6)[:, 0:768].rearrange("p (h t) -> p h t", h=6)
                    ptk = psbf(7)[:, 0:768].rearrange("p (h t) -> p h t", h=6)
                    for h in range(6):
                        tr(ptq[0:64, h, :], qs[i % 2][:, h, :], ident_b)
                    for h in range(6):
                        tr(ptk[0:64, h, :], ks[i % 2][:, h, :], ident_b)
                    cp("act", qa[0:64, :, tsl], ptq[0:64, :, :])
                    cp("dve", ka[0:64, :, tsl], ptk[0:64, :, :])
                return (fa, fb)
            for f in pipelined([p4a(i) for i in range(NT)], 1):
                f()
            for h in range(6):
                red(kms[0:64, h, :], ka[0:64, h, :].rearrange("p (n k) -> p n k", n=8), ALU.add)
            ts("dve", kmT[0:64, :, :], kms[0:64, :, :], 1.0 / 256.0, None, ALU.mult)
            def p4g(i):
                tsl = slice(i * 128, (i + 1) * 128)

                def fa():
                    pg = psb(i % 2, 0, 48).rearrange("p (h n) -> p h n", h=6)
                    for h in range(6):
                        mm(pg[:, h, :], qa[0:64, h, tsl], kmT[0:64, h, :])
                    g_ = gmk[i % 2]
                    tt("dve", g_, pg, padc[:, i, :].unsqueeze(1).to_broadcast([128, 6, 8]), ALU.add)
                    for h in range(6):
                        max8(g8[i % 2][:, h, :], g_[:, h, :])
                    tt("dve", g_, g_, g8[i % 2][:, :, 2:3].to_broadcast([128, 6, 8]), ALU.is_lt)
                    tt("dve", qb[i % 2][:, :, 64:72], g_, negm[:, i, :].unsqueeze(1).to_broadcast([128, 6, 8]), ALU.mult)

                def fb():
                    pb = psbf(2 + (i % 2))[:, 0:768].rearrange("p (h t) -> p h t", h=6)
                    for h in range(6):
                        tr(pb[0:72, h, :], qb[i % 2][:, h, :], ident_b)
                    cp("act", qa[64:72, :, tsl], pb[64:72, :, :])
                return (fa, fb)
            for f in pipelined([p4g(i) for i in range(NT)], 1):
                f()
            PT = [SA.bf(512) for _ in range(4)]
            rec = [SA.f32(512) for _ in range(2)]
            st4 = {"npt": 0, "nacc": 0}
            items = []
            for h in range(6):
                odd = h % 2
                for qg in range(4):
                    nch = 4 * qg + 4
                    grp = {}

                    def mk(h, qg, c, nch, grp):
                        st_ = {}

                        def fa():
                            if c == 0:
                                grp["ob"] = 4 + (st4["nacc"] % 2)
                                st4["nacc"] += 1
                            csl = slice(c * 128, (c + 1) * 128)
                            col0 = 0 if c < 4 * qg else 128 * (c - 4 * qg)
                            st_["col0"] = col0
                            st_["sbk"] = st4["npt"] % 4
                            st_["pt"] = PT[st4["npt"] % 4]
                            st4["npt"] += 1
                            diag = c >= 4 * qg
                            mm(psb(st_["sbk"], col0, 512), ka[0:72, h, csl], qa[0:72, h, qg * 512 + col0:(qg + 1) * 512], start=True, stop=not diag)
                            if diag:
                                mm(psb(st_["sbk"], col0, col0 + 128), ident_b, trit_b, start=False, stop=True)

                        def fb():
                            col0 = st_["col0"]
                            act(st_["pt"][:, col0:512], psb(st_["sbk"], col0, 512), AF.Exp, scale=0.125)
                            mm(psb(grp["ob"], col0, 512), vm[:, c, h, :], st_["pt"][:, col0:512], start=(c == 0), stop=(c == nch - 1))
                        return (fa, fb)

                    def mknorm(h, qg, grp, odd):
                        def fn():
                            ob = grp["ob"]
                            qsl = slice(qg * 512, (qg + 1) * 512)
                            r_ = rec[ob % 2]
                            if not odd:
                                recip(r_[0:64, :], psb(ob)[64:128, :])
                                tt("dve", yT[0:64, 5 + h // 2, qsl], psb(ob)[0:64, :], r_[0:64, :], ALU.mult)
                            else:
                                recip(r_[64:128, :], psb(ob)[0:64, :])
                                tt("dve", yT[64:128, 5 + h // 2, qsl], psb(ob)[64:128, :], r_[64:128, :], ALU.mult)
                        return (None, fn)
                    for c in range(nch):
                        items.append(mk(h, qg, c, nch, grp))
                    items.append(mknorm(h, qg, grp, odd))
            for f in pipelined(items, 2):
                f()

        def phase5(l, s, SA, xseq, gates, g1bc):
            xsrc = x_d if l == 0 else xs_d
            wo_b = SA.bf(KC * D).rearrange("p (k n) -> p k n", k=KC)
            stg = [SA.f32(D) for _ in range(2)]
            for kc in range(KC):
                dma("sp", stg[kc % 2], wo_d[l, kc * 128:(kc + 1) * 128, :])
                tt("dve", wo_b[:, kc, :], stg[kc % 2], g1bc, ALU.mult)
            xt = [SA.f32(D) for _ in range(2)]
            xhi = [SA.bf(D) for _ in range(2)]
            xlo = [SA.bf(D) for _ in range(2)]
            xTh = [SA.bf(D).rearrange("p (k t) -> p k t", k=KC) for _ in range(2)]
            xTl = [SA.bf(D).rearrange("p (k t) -> p k t", k=KC) for _ in range(2)]
            junk = SA.bf(D)
            st2 = SA.f32(4)
            wr3 = wrT.rearrange("p (k e) -> p k e", e=16)
            wrm = SA.f32(128).rearrange("p (k e) -> p k e", e=16)
            wrh3 = wr_hi.rearrange("p (k e) -> p k e", e=16)
            wrl3 = wr_lo.rearrange("p (k e) -> p k e", e=16)
            tt("dve", wrm, wr3, gm[:, 8:16].unsqueeze(2).to_broadcast([128, KC, 16]), ALU.mult)
            cp("dve", wrh3, wrm)
            tt("dve", wrm, wrm, wrh3, ALU.subtract)
            cp("dve", wrl3, wrm)
            for kc in range(KC):
                mm(psb(7, 0, 16)[0:1, :], sh2[:, kc:kc + 1], wr3[:, kc, :], start=(kc == 0), stop=(kc == KC - 1))
            cp("dve", brow[0:1, :], psb(7, 0, 16)[0:1, :])
            cp("dve", brow_hi[0:1, :], brow[0:1, :])
            tt("dve", brow[0:1, :], brow[0:1, :], brow_hi[0:1, :], ALU.subtract)
            cp("dve", brow_lo[0:1, :], brow[0:1, :])
            plog = psb(6, 0, NT * 16).rearrange("p (i e) -> p i e", e=16)

            def p5(i):
                tsl = slice(i * 128, (i + 1) * 128)
                b0 = (i % 2) * 2
                x_t = xt[i % 2]
                xs_i = xseq[:, i, :]
                ssum = st2[:, (i % 2) * 2:(i % 2) * 2 + 1]
                rstd = st2[:, (i % 2) * 2 + 1:(i % 2) * 2 + 2]
                pbh = psbf(4).rearrange("p (k t) -> p k t", k=KC)
                pbl = psbf(5).rearrange("p (k t) -> p k t", k=KC)

                def s1():
                    for nh in range(2):
                        for kc in range(KC):
                            mm(psb(b0 + nh), yT[:, kc, tsl], wo_b[:, kc, nh * 512:(nh + 1) * 512], start=(kc == 0), stop=(kc == KC - 1))
                    dma("sp", x_t, xsrc[s, i * 128:(i + 1) * 128, :])
                    for nh in range(2):
                        tt("dve", xs_i[:, nh * 512:(nh + 1) * 512], psb(b0 + nh), x_t[:, nh * 512:(nh + 1) * 512], ALU.add)

                def s2():
                    rms_rstd(xs_i, junk, ssum, rstd)
                    act(xhi[i % 2], xs_i, AF.Identity, scale=rstd)
                    stt(xlo[i % 2], xs_i, rstd, xhi[i % 2], ALU.mult, ALU.subtract)

                def s3():
                    for kc in range(KC):
                        tr(pbh[:, kc, :], xhi[i % 2][:, kc * 128:(kc + 1) * 128], ident_b)
                    for kc in range(KC):
                        tr(pbl[:, kc, :], xlo[i % 2][:, kc * 128:(kc + 1) * 128], ident_b)

                def s4():
                    e1 = "act" if i % 2 == 0 else "dve"
                    e2 = "dve" if i % 2 == 0 else "act"
                    cp(e1, xTh[i % 2], pbh)
                    cp(e2, xTl[i % 2], pbl)
                    for kc in range(KC):
                        if i % 2 == 0:
                            act(hT[:, kc, tsl], pbh[:, kc, :], AF.Identity, scale=gm[:, 8 + kc:9 + kc], bias=sh2[:, kc:kc + 1])
                        else:
                            ts("dve", hT[:, kc, tsl], pbh[:, kc, :], gm[:, 8 + kc:9 + kc], sh2[:, kc:kc + 1], ALU.mult, ALU.add)

                def s5():
                    for kc in range(KC):
                        mm(plog[:, i, :], xTh[i % 2][:, kc, :], wrh3[:, kc, :], start=(kc == 0), stop=False)
                        mm(plog[:, i, :], xTl[i % 2][:, kc, :], wrh3[:, kc, :], start=False, stop=False)
                        mm(plog[:, i, :], xTh[i % 2][:, kc, :], wrl3[:, kc, :], start=False, stop=False)
                    mm(plog[:, i, :], ones_b[0:1, :], brow_hi[0:1, :], start=False, stop=False)
                    mm(plog[:, i, :], ones_b[0:1, :], brow_lo[0:1, :], start=False, stop=True)
                return [s1, s2, s3, s4, s5]
            stages = [p5(i) for i in range(NT)]
            delays = [0, 0, 1, 1, 2]
            for t in range(NT + max(delays)):
                for j, dly in enumerate(delays):
                    k = t - dly
                    if 0 <= k < NT:
                        stages[k][j]()
            aff = SA.f32(NT * 16).rearrange("p (i e) -> p i e", e=16)
            bia = SA.f32(NT * 16).rearrange("p (i e) -> p i e", e=16)
            t16 = SA.f32(NT * 16).rearrange("p (i e) -> p i e", e=16)
            m1 = SA.f32(NT * 4)
            m2 = SA.f32(NT * 4)
            gs = SA.f32(NT * 4)
            gmx = SA.f32(NT)
            ing = SA.f32(NT * 4)
            ssm = SA.f32(NT)
            act(aff, plog, AF.Sigmoid)
            tt("dve", bia, aff, brt_bc.unsqueeze(1).to_broadcast([128, NT, 16]), ALU.add)
            bia4 = bia.rearrange("p i (g k) -> p (i g) k", g=4)
            t4 = t16.rearrange("p i (g k) -> p (i g) k", g=4)
            red(m1, bia4, ALU.max)
            tt("dve", t4, bia4, m1.unsqueeze(2).to_broadcast([128, NT * 4, 4]), ALU.is_equal)
            stt(t4, t4, -1.0e9, bia4, ALU.mult, ALU.add)
            red(m2, t4, ALU.max)
            tt("dve", gs, m1, m2, ALU.add)
            red(gmx, gs.rearrange("p (i g) -> p i g", g=4), ALU.max)
            tt("dve", ing.rearrange("p (i g) -> p i g", g=4), gs.rearrange("p (i g) -> p i g", g=4), gmx.unsqueeze(2).to_broadcast([128, NT, 4]), ALU.is_ge)
            tt("dve", t4, bia4, m2.unsqueeze(2).to_broadcast([128, NT * 4, 4]), ALU.is_ge)
            tt("dve", t4, t4, ing.unsqueeze(2).to_broadcast([128, NT * 4, 4]), ALU.mult)
            tt("dve", t16, t16, aff, ALU.mult)
            red(ssm, t16, ALU.add)
            recip(ssm, ssm)
            tt("dve", gates, t16, ssm.unsqueeze(2).to_broadcast([128, NT, 16]), ALU.mult)

        def phase6(l, s, SA, SB_, xseq, gates, g2bc):
            wg32 = [SA.f32(KC * DEXP).rearrange("p (k f) -> p k f", k=KC) for _ in range(2)]
            wu32 = [SA.f32(KC * DEXP).rearrange("p (k f) -> p k f", k=KC) for _ in range(2)]
            wd321 = SA.f32(2 * D).rearrange("p (c n) -> p c n", c=2)
            wd32 = [wd321, wd321]
            wgu = [SB_.bf(KC * 512).rearrange("p (k f) -> p k f", k=KC) for _ in range(2)]
            wdb = [SB_.bf(2 * D).rearrange("p (c n) -> p c n", c=2) for _ in range(2)]
            sgt = [SA.f32(512) for _ in range(2)]
            aT = [SB_.bf(2 * 512).rearrange("p (c t) -> p c t", c=2) for _ in range(2)]
            st6 = {"ndn": 0}
            items = []
            for e in range(NEXP):
                for tg in range(4):
                    def mk(e, tg):
                        p_ = e % 2
                        tsl = slice(tg * 512, (tg + 1) * 512)
                        a_ = aT[tg % 2]

                        def fa():
                            if tg == 0:
                                dma("sp", wg32[p_], wg_d[l, e].rearrange("(k p) f -> p k f", p=128))
                                dma("sp", wu32[p_], wu_d[l, e].rearrange("(k p) f -> p k f", p=128))
                                dma("sp", wd32[p_], wd_d[l, e].rearrange("(c p) n -> p c n", p=128))
                                cp("pool", wgu[p_][:, :, 0:256], wg32[p_])
                                cp("pool", wgu[p_][:, :, 256:512], wu32[p_])
                                tt("pool", wdb[p_], wd32[p_], g2bc.unsqueeze(1).to_broadcast([128, 2, D]), ALU.mult)
                            for half in range(2):
                                bg = half * 2
                                bu = half * 2 + 1
                                for kc in range(KC):
                                    mm(psb(bg), wgu[p_][:, kc, half * 128:(half + 1) * 128], hT[:, kc, tsl], start=(kc == 0), stop=(kc == KC - 1))
                                for kc in range(KC):
                                    mm(psb(bu), wgu[p_][:, kc, 256 + half * 128:256 + (half + 1) * 128], hT[:, kc, tsl], start=(kc == 0), stop=(kc == KC - 1))
                                act(sgt[half], psb(bg), AF.Silu)
                                tt("dve", a_[:, half, :], psb(bu), sgt[half], ALU.mult)

                        def fb():
                            for tl in range(4):
                                i = tg * 4 + tl
                                for nh in range(2):
                                    bank = 4 + (st6["ndn"] % 4)
                                    st6["ndn"] += 1
                                    for half in range(2):
                                        mm(psb(bank), a_[:, half, tl * 128:(tl + 1) * 128], wdb[p_][:, half, nh * 512:(nh + 1) * 512], start=(half == 0), stop=(half == 1))
                                    stt(xseq[:, i, nh * 512:(nh + 1) * 512], psb(bank), gates[:, i, e:e + 1], xseq[:, i, nh * 512:(nh + 1) * 512], ALU.mult, ALU.add)
                        return (fa, fb)
                    items.append(mk(e, tg))
            for f in pipelined(items, 1):
                f()

        def phase7(l, s, SA, xseq):
            if l < DEPTH - 1:
                dma("sp", xs_d[s].rearrange("(i p) d -> p i d", p=128), xseq)
                return
            gfb = SA.f32(D)
            dma("sp", gfb, gfin_d.partition_broadcast(128))
            junk = SA.bf(D)
            st2 = SA.f32(4)
            ot = [SA.f32(D) for _ in range(2)]
            for i in range(NT):
                ssum = st2[:, (i % 2) * 2:(i % 2) * 2 + 1]
                rstd = st2[:, (i % 2) * 2 + 1:(i % 2) * 2 + 2]
                rms_rstd(xseq[:, i, :], junk, ssum, rstd)
                stt(ot[i % 2], xseq[:, i, :], rstd, gfb, ALU.mult, ALU.mult)
                finals.append(dma("sp", out_d[s, i * 128:(i + 1) * 128, :], ot[i % 2]))

        def dump_bf(name, ap2d, n):
            if name in dbg_d:
                o = AW - n
                tmpf = sb[:, o:o + n]
                cp("dve", tmpf, ap2d)
                finals.append(dma("sp", dbg_d[name], tmpf))

        done = False
        if dbg:
            memset("pool", yT.rearrange("p k t -> p (k t)"), 0.0)
        if stop_after == "pro":
            layers = 0
        for l in range(layers):
            for s in range(2):
                first = (l == 0 and s == 0)
                load_vectors(l, s)
                if stop_after == "lv":
                    finals.append(dma("sp", dbg_d["hT"][:, 0:128], vecT))
                    done = True
                    break
                w2 = load_win(l, 0, 512, Alloc(AW - 4752, AW - 2704))
                w3 = load_win(l, 512, 1188, Alloc(AW - 2704, AW))
                phase1(l, s, Alloc(S0, AW - 4752))
                if first:
                    dump_bf("hT", hT.rearrange("p k t -> p (k t)"), KC * L)
                if stop_after == "p1":
                    done = True
                    break
                phase2(l, s, Alloc(S0, AW - 4752), w2)
                if stop_after == "p2":
                    if first:
                        dump_bf("yT", yT.rearrange("p k t -> p (k t)"), KC * L)
                    done = True
                    break
                phase3(l, s, Alloc(S0, AW), w3)
                if stop_after == "p3":
                    if first:
                        dump_bf("yT", yT.rearrange("p k t -> p (k t)"), KC * L)
                    done = True
                    break
                phase4(l, s, Alloc(S0, AW))
                if first and stop_after == "p4":
                    dump_bf("yT", yT.rearrange("p k t -> p (k t)"), KC * L)
                    done = True
                    break
                SA = Alloc(S0, AW)
                xseq = SA.f32(NT * D).rearrange("p (i d) -> p i d", i=NT)
                gates = SA.f32(NT * 16).rearrange("p (i e) -> p i e", e=16)
                gbc_ = [SA.f32(D), SA.f32(D)]
                dma("sp", gbc_[0], mod_d[l, s, 2 * D:3 * D].partition_broadcast(128))
                dma("sp", gbc_[1], mod_d[l, s, 5 * D:6 * D].partition_broadcast(128))
                m5 = SA.o
                phase5(l, s, SA, xseq, gates, gbc_[0])
                if first and "xmid" in dbg_d:
                    finals.append(dma("sp", dbg_d["xmid"].rearrange("(i p) d -> p i d", p=128), xseq))
                if first and "gates" in dbg_d:
                    finals.append(dma("sp", dbg_d["gates"], gates.rearrange("p i e -> p (i e)")))
                if first and "h2T" in dbg_d and stop_after == "p5":
                    dump_bf("h2T", hT.rearrange("p k t -> p (k t)"), KC * L)
                if stop_after == "p5":
                    done = True
                    break
                yo = region(yT)[1] // 4
                phase6(l, s, Alloc(m5, AW), Alloc(yo, yo + KC * L // 2), xseq, gates, gbc_[1])
                if first and "x1" in dbg_d:
                    finals.append(dma("sp", dbg_d["x1"].rearrange("(i p) d -> p i d", p=128), xseq))
                if stop_after == "p6":
                    done = True
                    break
                phase7(l, s, Alloc(m5, AW), xseq)
            if done:
                break
        print("S0 words", S0, "ops", len(P.ops))
        P.emit(nc, final_wait_ops=finals)
    return nc


_CACHE = {}


def kernel(**inputs):
    ctab = make_ctab()
    kind = make_kind()
    if "nc" not in _CACHE:
        _CACHE["nc"] = build_program()
    nc = _CACHE["nc"]
    in_maps = []
    shared = {k: np.ascontiguousarray(np.asarray(v, dtype=np.float32)) for k, v in inputs.items() if k not in ("x", "c")}
    x = np.asarray(inputs["x"], dtype=np.float32)
    c = np.asarray(inputs["c"], dtype=np.float32)
    for core in range(NCORES):
        m = dict(shared)
        m["x"] = np.ascontiguousarray(x[2 * core:2 * core + 2])
        m["c"] = np.ascontiguousarray(c[2 * core:2 * core + 2])
        m["ctab"] = ctab
        m["kind"] = kind
        in_maps.append(m)
    res = run_bass_kernel_spmd(nc, in_maps, core_ids=list(range(NCORES)))
    out = np.concatenate([np.asarray(r["out"]) for r in res.results], axis=0)
    return out.astype(np.float32)
```
